# Optimizing a Trainium2 kernel written in Bass

```python
import math
import jax, jax.numpy as jnp
from jax import lax
import numpy as np

D_MODEL = 2048
BATCH = 2
SEQ = 16384
DEPTH = 2

N_EVEN = (DEPTH + 1) // 2
N_ODD = DEPTH // 2

POOL_WIDTH = D_MODEL // 2
POOL_WINDOWS = (2, 4, 8, 16)
N_POOL_GROUPS = len(POOL_WINDOWS)
POOL_GROUP = POOL_WIDTH // N_POOL_GROUPS

CONV_WIDTH = D_MODEL - POOL_WIDTH
CONV_K = 3
IN_PROJ_WIDTH = POOL_WIDTH + 3 * CONV_WIDTH

N_HEADS = 16
HEAD_DIM = D_MODEL // N_HEADS
Q_BLOCK = 128
FOX_IN_WIDTH = 3 * D_MODEL + N_HEADS

N_EXPERTS = 32
TOP_K = 4
D_FF = D_MODEL
SWIGLU_ALPHA = 1.702
SWIGLU_LIMIT = 7.0
MOE_BLOCK = 256

LN_EPS = 1e-5
DEEPNORM_ALPHA = (2.0 * DEPTH) ** 0.25
DEEPNORM_BETA = (8.0 * DEPTH) ** -0.25

kernel_name = "hybrid_pool_conv_fox_moe_deepnorm"


def layer_norm(x, g, b):
    xf = x.astype(jnp.float32)
    mu = jnp.mean(xf, axis=-1, keepdims=True)
    xc = xf - mu
    var = jnp.mean(xc * xc, axis=-1, keepdims=True)
    y = xc * lax.rsqrt(var + LN_EPS)
    return (y * g.astype(jnp.float32) + b.astype(jnp.float32)).astype(x.dtype)


def pool_mixer(a, pool_w, pool_scale):
    bsz, s_len, _ = a.shape
    af = a.astype(jnp.float32)
    cs = jnp.pad(jnp.cumsum(af, axis=1), ((0, 0), (1, 0), (0, 0)))
    t = jnp.arange(s_len)
    outs = []
    for g, w in enumerate(POOL_WINDOWS):
        lo_c, hi_c = g * POOL_GROUP, (g + 1) * POOL_GROUP
        cs_g = cs[:, :, lo_c:hi_c]
        upper = cs_g[:, 1:, :]
        lower = jnp.pad(cs_g[:, : s_len + 1 - w, :], ((0, 0), (w - 1, 0), (0, 0)))
        count = jnp.minimum(t + 1, w).astype(jnp.float32)[None, :, None]
        outs.append((upper - lower) / count - af[:, :, lo_c:hi_c])
    p = jnp.stack(outs, axis=2).astype(a.dtype)
    y = jnp.einsum('bsgc,gce->bsge', p, pool_w).reshape(bsz, s_len, POOL_WIDTH)
    return y * pool_scale


def short_conv_mixer(gate_b, gate_c, v, conv_w):
    z = gate_c * v
    y = lax.conv_general_dilated(
        z, conv_w[:, None, :], window_strides=(1,), padding=[(CONV_K - 1, 0)],
        dimension_numbers=('NWC', 'WIO', 'NWC'), feature_group_count=CONV_WIDTH)
    return gate_b * y


def pool_conv_layer_mixer(x, w_in, pool_w, pool_scale, conv_w, w_out):
    h = jnp.einsum('bsd,de->bse', x, w_in)
    a = h[..., :POOL_WIDTH]
    gate_b = h[..., POOL_WIDTH:POOL_WIDTH + CONV_WIDTH]
    gate_c = h[..., POOL_WIDTH + CONV_WIDTH:POOL_WIDTH + 2 * CONV_WIDTH]
    v = h[..., POOL_WIDTH + 2 * CONV_WIDTH:]
    y = jnp.concatenate([pool_mixer(a, pool_w, pool_scale),
                         short_conv_mixer(gate_b, gate_c, v, conv_w)], axis=-1)
    return jnp.einsum('bse,ed->bsd', y, w_out)


def forgetting_attention(x, w_in, b_f, w_o):
    bsz, s_len, _ = x.shape
    n_blocks = s_len // Q_BLOCK
    h = jnp.einsum('bsd,de->bse', x, w_in)
    q = h[..., :D_MODEL].reshape(bsz, s_len, N_HEADS, HEAD_DIM) * (HEAD_DIM ** -0.5)
    k = h[..., D_MODEL:2 * D_MODEL].reshape(bsz, s_len, N_HEADS, HEAD_DIM)
    v = h[..., 2 * D_MODEL:3 * D_MODEL].reshape(bsz, s_len, N_HEADS, HEAD_DIM)
    log_f = jax.nn.log_sigmoid((h[..., 3 * D_MODEL:] + b_f).astype(jnp.float32))
    c = jnp.cumsum(log_f, axis=1).transpose(0, 2, 1)
    q_blocks = q.reshape(bsz, n_blocks, Q_BLOCK, N_HEADS, HEAD_DIM).transpose(1, 0, 2, 3, 4)
    c_blocks = c.reshape(bsz, N_HEADS, n_blocks, Q_BLOCK).transpose(2, 0, 1, 3)
    k_pos = jnp.arange(s_len)

    def attend_block(args):
        q_blk, c_q, blk = args
        q_pos = blk * Q_BLOCK + jnp.arange(Q_BLOCK)
        logits = jnp.einsum('bqhd,bkhd->bhqk', q_blk, k).astype(jnp.float32)
        logits = logits + c_q[..., :, None] - c[:, :, None, :]
        causal = k_pos[None, :] <= q_pos[:, None]
        logits = jnp.where(causal, logits, -jnp.inf)
        probs = jax.nn.softmax(logits, axis=-1).astype(v.dtype)
        return jnp.einsum('bhqk,bkhd->bqhd', probs, v)

    o = lax.map(attend_block, (q_blocks, c_blocks, jnp.arange(n_blocks)))
    o = o.transpose(1, 0, 2, 3, 4).reshape(bsz, s_len, D_MODEL)
    return jnp.einsum('bse,ed->bsd', o, w_o)


def moe_ffn(x, router_w, router_b, w_gu, b_gu, w_down, b_down):
    bsz, s_len, d = x.shape
    n_tok = bsz * s_len
    xt = x.reshape(n_tok, d)
    logits = (jnp.einsum('td,de->te', xt, router_w) + router_b).astype(jnp.float32)
    top_v, top_e = lax.top_k(logits, TOP_K)
    gates = jax.nn.softmax(top_v, axis=-1)

    n_assign = n_tok * TOP_K
    flat_e = top_e.reshape(-1)
    flat_tok = jnp.arange(n_assign, dtype=jnp.int32) // TOP_K
    order = jnp.argsort(flat_e)
    e_sorted = flat_e[order]
    counts = jnp.bincount(flat_e, length=N_EXPERTS)
    padded = (counts + MOE_BLOCK - 1) // MOE_BLOCK * MOE_BLOCK
    pad_end = jnp.cumsum(padded)
    pad_start = pad_end - padded
    start = jnp.cumsum(counts) - counts
    dest = pad_start[e_sorted] + jnp.arange(n_assign, dtype=jnp.int32) - start[e_sorted]
    n_blocks = -(-(n_assign + N_EXPERTS * (MOE_BLOCK - 1)) // MOE_BLOCK)
    n_slots = n_blocks * MOE_BLOCK
    slot_tok = jnp.full((n_slots,), n_tok, jnp.int32).at[dest].set(flat_tok[order])
    slot_gate = jnp.zeros((n_slots,), jnp.float32).at[dest].set(gates.reshape(-1)[order])
    block_e = jnp.minimum(
        jnp.searchsorted(pad_end, jnp.arange(n_blocks, dtype=jnp.int32) * MOE_BLOCK, side='right'),
        N_EXPERTS - 1)
    x_pad = jnp.concatenate([xt, jnp.zeros((1, d), xt.dtype)], axis=0)

    def expert_block(args):
        tok, e = args
        xb = x_pad[tok]
        gu = xb @ w_gu[e] + b_gu[e]
        x_glu = jnp.minimum(gu[:, :D_FF], SWIGLU_LIMIT)
        x_lin = jnp.clip(gu[:, D_FF:], -SWIGLU_LIMIT, SWIGLU_LIMIT)
        hid = (x_lin + 1.0) * (x_glu * jax.nn.sigmoid(SWIGLU_ALPHA * x_glu))
        return hid @ w_down[e] + b_down[e]

    yb = lax.map(expert_block, (slot_tok.reshape(n_blocks, MOE_BLOCK), block_e))
    y = yb.reshape(n_slots, d) * slot_gate[:, None].astype(yb.dtype)
    out = jnp.zeros((n_tok + 1, d), y.dtype).at[slot_tok].add(y)[:n_tok]
    return out.reshape(bsz, s_len, d)


def setup_inputs(seed: int = 0) -> dict:
    key = jax.random.key(seed)
    ks = jax.random.split(key, 24)
    nrm = jax.random.normal
    f32 = jnp.float32
    d = D_MODEL
    return {
        "x": nrm(ks[0], (BATCH, SEQ, d), f32),
        "ab_w_in": nrm(ks[1], (N_EVEN, d, IN_PROJ_WIDTH), f32) * d ** -0.5,
        "ab_pool_w": nrm(ks[2], (N_EVEN, N_POOL_GROUPS, POOL_GROUP, POOL_GROUP), f32) * POOL_GROUP ** -0.5,
        "ab_pool_scale": 1.0 + 0.1 * nrm(ks[3], (N_EVEN, POOL_WIDTH), f32),
        "ab_conv_w": nrm(ks[4], (N_EVEN, CONV_K, CONV_WIDTH), f32) * CONV_K ** -0.5,
        "ab_w_out": nrm(ks[5], (N_EVEN, POOL_WIDTH + CONV_WIDTH, d), f32) * (POOL_WIDTH + CONV_WIDTH) ** -0.5 * DEEPNORM_BETA,
        "fox_w_in": nrm(ks[6], (N_ODD, d, FOX_IN_WIDTH), f32) * d ** -0.5,
        "fox_b_f": jnp.linspace(1.0, 6.0, N_HEADS, dtype=f32)[None, :] + 0.1 * nrm(ks[7], (N_ODD, N_HEADS), f32),
        "fox_w_o": nrm(ks[8], (N_ODD, d, d), f32) * d ** -0.5 * DEEPNORM_BETA,
        "ln1_g": 1.0 + 0.05 * nrm(ks[9], (DEPTH, d), f32),
        "ln1_b": 0.02 * nrm(ks[10], (DEPTH, d), f32),
        "ln2_g": 1.0 + 0.05 * nrm(ks[11], (DEPTH, d), f32),
        "ln2_b": 0.02 * nrm(ks[12], (DEPTH, d), f32),
        "router_w": nrm(ks[13], (DEPTH, d, N_EXPERTS), f32) * d ** -0.5,
        "router_b": 0.01 * nrm(ks[14], (DEPTH, N_EXPERTS), f32),
        "w_gu": nrm(ks[15], (DEPTH, N_EXPERTS, d, 2 * D_FF), f32) * d ** -0.5,
        "b_gu": 0.01 * nrm(ks[16], (DEPTH, N_EXPERTS, 2 * D_FF), f32),
        "w_down": nrm(ks[17], (DEPTH, N_EXPERTS, D_FF, d), f32) * D_FF ** -0.5 * DEEPNORM_BETA,
        "b_down": 0.01 * nrm(ks[18], (DEPTH, N_EXPERTS, d), f32),
    }


def reference(x, ab_w_in, ab_pool_w, ab_pool_scale, ab_conv_w, ab_w_out,
              fox_w_in, fox_b_f, fox_w_o,
              ln1_g, ln1_b, ln2_g, ln2_b,
              router_w, router_b, w_gu, b_gu, w_down, b_down):
    for layer in range(DEPTH):
        j = layer // 2
        if layer % 2 == 0:
            mix = pool_conv_layer_mixer(x, ab_w_in[j], ab_pool_w[j], ab_pool_scale[j],
                                        ab_conv_w[j], ab_w_out[j])
        else:
            mix = forgetting_attention(x, fox_w_in[j], fox_b_f[j], fox_w_o[j])
        x = layer_norm(DEEPNORM_ALPHA * x + mix, ln1_g[layer], ln1_b[layer])
        ffn = moe_ffn(x, router_w[layer], router_b[layer], w_gu[layer], b_gu[layer],
                      w_down[layer], b_down[layer])
        x = layer_norm(DEEPNORM_ALPHA * x + ffn, ln2_g[layer], ln2_b[layer])
    return x
```

```python
from contextlib import ExitStack
import numpy as np
import concourse.bass as bass
import concourse.mybir as mybir

F32 = mybir.dt.float32
BF16 = mybir.dt.bfloat16
I32 = mybir.dt.int32
U32 = mybir.dt.uint32
ALU = mybir.AluOpType
AF = mybir.ActivationFunctionType
AX = mybir.AxisListType


class Buf:
    __slots__ = ("name", "w", "r")

    def __init__(self, name=""):
        self.name = name
        self.w = None
        self.r = {}


class KB:
    ENG = ("pe", "act", "dve", "pool", "sp")

    def __init__(self, nc, ndma_sems=24):
        self.nc = nc
        self.h = {"pe": nc.tensor, "act": nc.scalar, "dve": nc.vector, "pool": nc.gpsimd, "sp": nc.sync}
        self.prog = {e: [] for e in self.ENG}
        self.sem = {}
        self.cnt = {e: 0 for e in self.ENG}
        self.waited = {e: {} for e in self.ENG}
        self._cm = []
        for e in self.ENG:
            cm = nc.semaphore("s_" + e)
            self.sem[e] = cm.__enter__()
            self._cm.append(cm)
        self.dsem = []
        self.dcnt = []
        for i in range(ndma_sems):
            cm = nc.semaphore("d%d" % i)
            self.dsem.append(cm.__enter__())
            self._cm.append(cm)
            self.dcnt.append(0)
        self.dnext = 0
        self.same_engine_sync = True
        self.n_inst = 0

    def _wait(self, eng, tok):
        if tok is None:
            return
        sem, val, src = tok
        if src == eng and (eng == "pe" or not self.same_engine_sync):
            return
        key = id(sem)
        if self.waited[eng].get(key, 0) >= val:
            return
        self.waited[eng][key] = val
        h = self.h[eng]
        self.prog[eng].append(lambda: h.wait_ge(sem, val))

    def _deps(self, eng, reads, writes):
        for b in reads:
            self._wait(eng, b.w)
        for b in writes:
            self._wait(eng, b.w)
            for t in b.r.values():
                self._wait(eng, t)

    def _commit(self, tok, reads, writes):
        for b in reads:
            o = b.r.get(id(tok[0]))
            if o is None or o[1] < tok[1]:
                b.r[id(tok[0])] = tok
        for b in writes:
            b.w = tok
            b.r = {}

    def op(self, eng, fn, reads=(), writes=(), inc=True):
        self._deps(eng, reads, writes)
        h = self.h[eng]
        sem = self.sem[eng]
        self.n_inst += 1
        if inc:
            self.cnt[eng] += 1
            self.prog[eng].append(lambda: fn(h).then_inc(sem, 1))
            tok = (sem, self.cnt[eng], eng)
        else:
            self.prog[eng].append(lambda: fn(h))
            tok = (sem, self.cnt[eng] + 1, eng)
        self._commit(tok, reads, writes)
        return tok

    def dma(self, eng, fn, reads=(), writes=()):
        self._deps(eng, reads, writes)
        i = self.dnext
        self.dnext = (self.dnext + 1) % len(self.dsem)
        sem = self.dsem[i]
        self.dcnt[i] += 16
        val = self.dcnt[i]
        h = self.h[eng]
        self.n_inst += 1
        self.prog[eng].append(lambda: fn(h).then_inc(sem, 16))
        tok = (sem, val, "dma")
        self._commit(tok, reads, writes)
        return tok

    def pool_const_reg(self, val):
        if not hasattr(self, "_regs"):
            self._regs = {}
        if val not in self._regs:
            holder = {}
            self._regs[val] = holder
            hp = self.h["pool"]
            self.prog["pool"].append(lambda: holder.__setitem__("r", hp.to_reg(val)))
        return self._regs[val]

    def wait_tok(self, eng, tok):
        self._wait(eng, tok)

    def emit(self):
        nc = self.nc
        with nc.Block() as block:
            @block.tensor
            def _(e):
                for f in self.prog["pe"]:
                    f()

            @block.scalar
            def _(e):
                for f in self.prog["act"]:
                    f()

            @block.vector
            def _(e):
                for f in self.prog["dve"]:
                    f()

            @block.gpsimd
            def _(e):
                for f in self.prog["pool"]:
                    f()

            @block.sync
            def _(e):
                for f in self.prog["sp"]:
                    f()
        self.prog = {e: [] for e in self.ENG}

    def close(self):
        for cm in reversed(self._cm):
            cm.__exit__(None, None, None)


D = 2048
NT = 4096
TT = 256
NSUB = NT // 128
NE = 32
CAP = 768
CQ = CAP // 128
SG = CAP // 2
ALPHA = float(4.0 ** 0.25)
LN_EPS = 1e-5
BIG = 1.0e6


class MK:
    def __init__(self, nc):
        self.nc = nc
        self.k = KB(nc, ndma_sems=32)
        self.es = ExitStack()

    def dram_in(self, name, shape, dt):
        return self.nc.dram_tensor(name, list(shape), dt, kind="ExternalInput").ap()

    def dram_out(self, name, shape, dt):
        return self.nc.dram_tensor(name, list(shape), dt, kind="ExternalOutput").ap()

    def dram_scr(self, name, shape, dt):
        return self.nc.dram_tensor(name, list(shape), dt, kind="Internal").ap()


def alloc_fns(nc, es):
    def sb(name, shape, dt):
        _UID[0] += 1
        return es.enter_context(nc.sbuf_tensor("s%d_%s" % (_UID[0], name), list(shape), dt))

    def ps(name, shape, dt):
        _UID[0] += 1
        return es.enter_context(nc.psum_tensor("p%d_%s" % (_UID[0], name), list(shape), dt))
    return sb, ps


_UID = [0]


def barrier(k):
    for e in k.ENG:
        for e2 in k.ENG:
            if e2 != e and k.cnt[e2]:
                k.wait_tok(e, (k.sem[e2], k.cnt[e2], e2))
        for i, s in enumerate(k.dsem):
            if k.dcnt[i]:
                k.wait_tok(e, (s, k.dcnt[i], "dma"))


def load_consts(m, sb):
    nc, k = m.nc, m.k
    c = {}
    c["ident_f"] = sb("ident_f", [128, 128], F32)
    c["ident_bf"] = sb("ident_bf", [128, 128], BF16)
    c["lstrict"] = sb("lstrict", [128, 128], BF16)
    c["ones_bf"] = sb("ones_bf", [128, 128], BF16)
    c["iota32"] = sb("iota32", [128, 32], F32)
    c["tokid"] = sb("tokid", [128, NSUB, 2], I32)
    c["zero_i"] = sb("zero_i", [128, NE * CQ * 2], I32)
    c["destg"] = sb("destg", [128, NSUB, 4], I32)
    c["g4"] = sb("g4", [128, NSUB, 4], F32)
    c["cmask"] = sb("cmask", [128, 32], BF16)
    c["eps"] = sb("eps", [128, 1], F32)
    B = {n: Buf(n) for n in c}
    c["B"] = B
    d = m.d
    k.dma("sp", lambda h: h.dma_start(out=c["ident_f"][:], in_=d["ident"]), writes=[B["ident_f"]])
    k.dma("pool", lambda h: h.dma_start(out=c["ident_bf"][:], in_=d["ident"]), writes=[B["ident_bf"]])
    k.dma("pool", lambda h: h.dma_start(out=c["lstrict"][:], in_=d["lstrict"]), writes=[B["lstrict"]])
    k.dma("sp", lambda h: h.dma_start(out=c["iota32"][:], in_=d["iota32"]), writes=[B["iota32"]])
    k.dma("sp", lambda h: h.dma_start(out=c["tokid"][:], in_=d["tokid"].rearrange("p (a b) -> p a b", b=2)), writes=[B["tokid"]])
    k.op("dve", lambda h: h.memset(c["ones_bf"][:], 1.0), writes=[B["ones_bf"]])
    k.op("dve", lambda h: h.memset(c["zero_i"][:], 0), writes=[B["zero_i"]])
    k.op("dve", lambda h: h.memset(c["eps"][:], LN_EPS), writes=[B["eps"]])
    return c


class PostMixer:
    def __init__(self, m, sb, ps, layer, tps, TPS, lps, pps, MISC):
        self.m = m
        nc, k, d = m.nc, m.k, m.d
        self.layer = layer
        self.tps, self.TPS = tps, TPS
        self.lng = sb("lng", [128, D], F32)
        self.lnb = sb("lnb", [128, D], F32)
        self.rw = sb("rw", [128, 16, 32], F32)
        self.rb = sb("rb", [128, 32], F32)
        self.xmT = sb("xmT", [128, 16, 128], F32)
        self.st = sb("pm_st", [128, 4, 6], F32)
        self.sm = sb("pm_sm", [128, 96], F32)
        self.smi = sb("pm_smi", [128, 16], U32)
        self.dsc = sb("pm_dsc", [128, 4], I32)
        self.mask = sb("pm_mask", [128, 32], BF16)
        self.oh = sb("pm_oh", [128, 32], F32)
        self.lps = lps
        self.pps = pps
        self.Bp = Buf("lnparams")
        self.BxmT = Buf(); self.Bxmbf = Buf(); self.Bsm = Buf(); self.Blps = MISC; self.Bpps = MISC
        L = layer
        k.dma("sp", lambda h: h.dma_start(out=self.lng[:], in_=d["ln1_g"][L:L + 1, :].partition_broadcast(128)), writes=[self.Bp])
        k.dma("sp", lambda h: h.dma_start(out=self.lnb[:], in_=d["ln1_b"][L:L + 1, :].partition_broadcast(128)), writes=[self.Bp])
        k.dma("sp", lambda h: h.dma_start(out=self.rw[:], in_=d["router_w"][L].rearrange("(c p) e -> p c e", p=128)), writes=[self.Bp])
        k.dma("sp", lambda h: h.dma_start(out=self.rb[:], in_=d["router_b"][L:L + 1, :].partition_broadcast(128)), writes=[self.Bp])
        c = m.c
        k.op("dve", lambda h: h.memset(c["cmask"][:], 0.0), writes=[c["B"]["cmask"]])
        k.dma("sp", lambda h: h.dma_start(out=d["tok_list"].rearrange("(p x) o -> p (x o)", p=128), in_=c["zero_i"][:]),
              reads=[c["B"]["zero_i"]], writes=[m.Btoklist])

    def run(self, r, R, sub):
        m = self.m
        nc, k, d, c = m.nc, m.k, m.d, m.c
        CB = c["B"]
        sm, st = self.sm, self.st
        Bsm = self.Bsm
        l0 = sub * 128
        for n in range(4):
            k.op("dve", lambda h, n=n: h.bn_stats(out=st[:, n, :], in_=r[:, n * 512:(n + 1) * 512]), reads=[R], writes=[Bsm])
        k.op("dve", lambda h: h.bn_aggr(out=sm[:, 0:2], in_=st[:, :, :]), reads=[Bsm], writes=[Bsm])
        k.op("act", lambda h: h.activation(out=sm[:, 6:7], in_=sm[:, 1:2], func=AF.Ln, bias=c["eps"][:], scale=1.0), reads=[Bsm, CB["eps"]], writes=[Bsm])
        k.op("act", lambda h: h.activation(out=sm[:, 2:3], in_=sm[:, 6:7], func=AF.Exp, scale=-0.5), reads=[Bsm], writes=[Bsm])
        k.op("dve", lambda h: h.tensor_scalar(out=r, in0=r, scalar1=sm[:, 0:1], scalar2=sm[:, 2:3], op0=ALU.subtract, op1=ALU.mult), reads=[R, Bsm], writes=[R])
        k.op("dve", lambda h: h.tensor_tensor(out=r, in0=r, in1=self.lng[:], op=ALU.mult), reads=[R, self.Bp], writes=[R])
        k.op("dve", lambda h: h.tensor_tensor(out=r, in0=r, in1=self.lnb[:], op=ALU.add), reads=[R, self.Bp], writes=[R])
        k.dma("sp", lambda h: h.dma_start(out=d["xm_f32"][l0:l0 + 128, :], in_=r), reads=[R], writes=[Buf()])
        k.dma("pool", lambda h: h.dma_start(out=d["xm_bf"][l0:l0 + 128, :], in_=r), reads=[R], writes=[Buf()])
        for q in range(4):
            tp, TP = self.tps[q % 2], self.TPS[q % 2]
            for j in range(4):
                dc = q * 4 + j
                k.op("pe", lambda h, dc=dc, j=j, tp=tp: h.transpose(tp[:, j, :], r[:, dc * 128:(dc + 1) * 128], c["ident_f"][:]),
                     reads=[R, CB["ident_f"]], writes=[TP])
            if q % 2 == 0:
                k.op("act", lambda h, q=q, tp=tp: h.activation(out=self.xmT[:, q * 4:q * 4 + 4, :], in_=tp[:, :, :], func=AF.Copy), reads=[TP], writes=[self.BxmT])
            else:
                k.op("dve", lambda h, q=q, tp=tp: h.tensor_copy(out=self.xmT[:, q * 4:q * 4 + 4, :], in_=tp[:, :, :]), reads=[TP], writes=[self.BxmT])
        for dc in range(16):
            k.op("pe", lambda h, dc=dc: h.matmul(self.lps, lhsT=self.xmT[:, dc, :], rhs=self.rw[:, dc, :], start=(dc == 0), stop=(dc == 15)),
                 reads=[self.BxmT, self.Bp], writes=[self.Blps], inc=(dc == 15))
        lg = sm[:, 8:40]
        k.op("dve", lambda h: h.tensor_tensor(out=lg, in0=self.lps, in1=self.rb[:], op=ALU.add), reads=[self.Blps, self.Bp], writes=[Bsm])
        mx = sm[:, 40:48]
        k.op("dve", lambda h: h.max(out=mx, in_=lg), reads=[Bsm], writes=[Bsm])
        k.op("dve", lambda h: h.max_index(out=self.smi[:, 0:8], in_max=mx, in_values=lg), reads=[Bsm], writes=[Bsm])
        k.op("dve", lambda h: h.tensor_scalar(out=sm[:, 3:4], in0=mx[:, 0:1], scalar1=-1.0, scalar2=None, op0=ALU.mult), reads=[Bsm], writes=[Bsm])
        e4 = sm[:, 48:52]
        k.op("act", lambda h: h.activation(out=e4, in_=mx[:, 0:4], func=AF.Exp, bias=sm[:, 3:4], scale=1.0), reads=[Bsm], writes=[Bsm])
        k.op("dve", lambda h: h.reduce_sum(out=sm[:, 4:5], in_=e4, axis=AX.X), reads=[Bsm], writes=[Bsm])
        k.op("dve", lambda h: h.reciprocal(out=sm[:, 5:6], in_=sm[:, 4:5]), reads=[Bsm], writes=[Bsm])
        g4 = sm[:, 52:56]
        k.op("dve", lambda h: h.tensor_scalar(out=g4, in0=e4, scalar1=sm[:, 5:6], scalar2=None, op0=ALU.mult), reads=[Bsm], writes=[Bsm])
        k.op("dve", lambda h: h.tensor_scalar(out=self.mask[:], in0=lg, scalar1=mx[:, 3:4], scalar2=None, op0=ALU.is_ge), reads=[Bsm], writes=[Bsm])
        k.op("pe", lambda h: h.matmul(self.pps, lhsT=c["lstrict"][:], rhs=self.mask[:], start=True, stop=False),
             reads=[Bsm, CB["lstrict"]], writes=[self.Bpps], inc=False)
        k.op("pe", lambda h: h.matmul(self.pps, lhsT=c["ones_bf"][:], rhs=c["cmask"][:], start=False, stop=True),
             reads=[CB["cmask"], CB["ones_bf"]], writes=[self.Bpps])
        k.op("dve", lambda h: h.tensor_tensor(out=c["cmask"][:], in0=c["cmask"][:], in1=self.mask[:], op=ALU.add), reads=[Bsm], writes=[CB["cmask"]])
        posf = sm[:, 56:88]
        k.op("dve", lambda h: h.tensor_copy(out=posf, in_=self.pps), reads=[self.Bpps], writes=[Bsm])
        idxf = sm[:, 88:92]
        k.op("dve", lambda h: h.tensor_copy(out=idxf, in_=self.smi[:, 0:4]), reads=[Bsm], writes=[Bsm])
        pos4 = sm[:, 92:96]
        for kk in range(4):
            k.op("dve", lambda h, kk=kk: h.tensor_scalar(out=self.oh[:], in0=c["iota32"][:], scalar1=idxf[:, kk:kk + 1], scalar2=None, op0=ALU.is_equal),
                 reads=[Bsm, CB["iota32"]], writes=[Bsm])
            k.op("dve", lambda h: h.tensor_tensor(out=self.oh[:], in0=self.oh[:], in1=posf, op=ALU.mult), reads=[Bsm], writes=[Bsm])
            k.op("dve", lambda h, kk=kk: h.reduce_sum(out=pos4[:, kk:kk + 1], in_=self.oh[:], axis=AX.X), reads=[Bsm], writes=[Bsm])
        valid = sm[:, 6:8]
        valid = self.oh[:, 0:4]
        destf = self.oh[:, 4:8]
        tmpb = self.oh[:, 8:12]
        k.op("dve", lambda h: h.tensor_scalar(out=valid, in0=pos4, scalar1=float(CAP), scalar2=None, op0=ALU.is_lt), reads=[Bsm], writes=[Bsm])
        k.op("dve", lambda h: h.scalar_tensor_tensor(out=destf, in0=idxf, scalar=float(CAP), in1=pos4, op0=ALU.mult, op1=ALU.add), reads=[Bsm], writes=[Bsm])
        k.op("dve", lambda h: h.tensor_tensor(out=c["destg"][:, sub, :], in0=destf, in1=valid, op=ALU.mult), reads=[Bsm], writes=[CB["destg"]])
        k.op("dve", lambda h: h.tensor_tensor(out=c["g4"][:, sub, :], in0=g4, in1=valid, op=ALU.mult), reads=[Bsm], writes=[CB["g4"]])
        k.op("dve", lambda h: h.tensor_scalar(out=tmpb, in0=valid, scalar1=-BIG, scalar2=BIG, op0=ALU.mult, op1=ALU.add), reads=[Bsm], writes=[Bsm])
        k.op("dve", lambda h: h.tensor_tensor(out=self.dsc[:], in0=destf, in1=tmpb, op=ALU.add), reads=[Bsm], writes=[Bsm])
        breg = k.pool_const_reg(NE * CAP - 1)
        for kk in range(4):
            k.dma("pool", lambda h, kk=kk: h.indirect_dma_start(
                out=d["tok_list"], out_offset=bass.IndirectOffsetOnAxis(ap=self.dsc[:, kk:kk + 1], axis=0),
                in_=c["tokid"][:, sub, :], in_offset=None, bounds_check=breg["r"], oob_is_err=False),
                reads=[Bsm, CB["tokid"], m.Btoklist], writes=[Buf()])


def phase_A(m, blk):
    nc, k, d, c = m.nc, m.k, m.d, m.c
    CB = c["B"]
    with ExitStack() as es:
        sb, ps = alloc_fns(nc, es)
        x_tok = sb("x_tok", [128, 2, D], F32)
        xT = sb("xT", [128, 16, TT], BF16)
        xTh = sb("xTh", [128, 16, 16], BF16)
        wslab = [sb("wslab%d" % i, [128, 16, 512], BF16) for i in range(2)]
        A_t = sb("A_t", [128, 8, 16 + TT], F32)
        Z_t = sb("Z_t", [128, 8, 16 + TT], F32)
        GB = sb("GB", [128, 8, TT], F32)
        GC = sb("GC", [128, 2, TT], F32)
        GCh = sb("GCh", [128, 2, 16], F32)
        sA = sb("sA", [128, 2, 16 + TT], F32)
        sB = sb("sB", [128, 2, 16 + TT], F32)
        ct = [sb("ct%d" % i, [128, TT], F32) for i in range(2)]
        t16 = sb("t16", [128, 16], F32)
        pT = sb("pT", [128, 8, TT], BF16)
        yT = sb("yT", [128, 16, TT], BF16)
        wout = sb("wout", [128, 16, D], BF16)
        poolw = sb("poolw", [128, 4, 2, 256], BF16)
        pscale = sb("pscale", [128, 8], F32)
        cw = sb("cw", [128, 3, 8], F32)
        invc = sb("invc", [128, 2, 4, 16], F32)
        bank = [ps("bank%d" % i, [128, 512], F32) for i in range(8)]
        hps = [bank[0][:, 0:256], bank[0][:, 256:512], bank[1][:, 0:256], bank[1][:, 256:512]]
        tps = [bank[2][:, :].rearrange("p (j t) -> p j t", j=4), bank[3][:, :].rearrange("p (j t) -> p j t", j=4)]
        ops = [bank[4], bank[5]]
        pps2 = [bank[6][:, 0:256], bank[6][:, 256:512]]
        hhps = bank[7][:, 0:32].rearrange("p (j t) -> p j t", j=2)
        TPS = [Buf(), Buf()]
        MISC = Buf("miscbank")
        pm = PostMixer(m, sb, ps, 0, tps, TPS, bank[7][:, 32:64], bank[7][:, 64:96], MISC)
        Bw = Buf("Aweights")
        Bx = Buf("x_tok"); BxT = Buf("xT"); BxTh = Buf()
        xh = pm.xmT[0:16, :, :].rearrange("p a b -> p (a b)")
        Bxh = pm.BxmT
        BWS = [Buf(), Buf()]
        BA = Buf("A_t"); BZ = Buf("Z_t"); BGB = Buf(); BGC = Buf(); BGCh = Buf()
        BsA = Buf(); BsB = Buf(); Bct = [Buf(), Buf()]; Bt16 = Buf()
        BpT = Buf(); ByT = Buf()
        HB0, HB1 = Buf(), Buf()
        HPS = [HB0, HB0, HB1, HB1]; HHPS = MISC; OPS = [Buf(), Buf()]; PB = Buf(); PPS2 = [PB, PB]
        k.dma("pool", lambda h: h.dma_start(out=wout[:], in_=d["ab_w_out"].rearrange("(c p) f -> p c f", p=128)), writes=[Bw])
        k.dma("pool", lambda h: h.dma_start(out=poolw[:], in_=d["ab_pool_w"].rearrange("g (c p) e -> p g c e", p=128)), writes=[Bw])
        k.dma("sp", lambda h: h.dma_start(out=pscale[:], in_=d["pscale_t"]), writes=[Bw])
        k.dma("sp", lambda h: h.dma_start(out=cw[:], in_=d["cw_t"]), writes=[Bw])
        k.dma("sp", lambda h: h.dma_start(out=invc[:], in_=d["invc"]), writes=[Bw])
        k.op("dve", lambda h: h.memset(A_t[:], 0.0), writes=[BA])
        k.op("dve", lambda h: h.memset(Z_t[:], 0.0), writes=[BZ])
        w_in_v = d["ab_w_in"].rearrange("(c p) f -> p c f", p=128)
        slabs = []
        for s_ in range(4):
            slabs.append([(0, s_ * 512, 512)])
        for cs in range(4):
            slabs.append([(0, 2048 + cs * 256, 256), (256, 3072 + cs * 256, 256)])
        nslab_total = 0
        ntiles = NT // TT
        issued = {"n": 0}

        def issue_slabs_upto(nmax):
            while issued["n"] <= nmax and issued["n"] < ntiles * len(slabs):
                n_ = issued["n"]
                issued["n"] += 1
                wb_ = n_ % 2
                for (dc0, sc0, wd) in slabs[n_ % len(slabs)]:
                    k.dma("pool", lambda h, wb_=wb_, dc0=dc0, sc0=sc0, wd=wd: h.dma_start(out=wslab[wb_][:, :, dc0:dc0 + wd], in_=w_in_v[:, :, sc0:sc0 + wd]), writes=[BWS[wb_]])
        for ti in range(ntiles):
            ch = 0 if blk == 0 else 1
            first = (ti == 0)
            l0 = blk * NT + ti * TT
            k.dma("sp", lambda h, l0=l0: h.dma_start(out=x_tok[:], in_=d["xs"][l0:l0 + TT, :].rearrange("(s p) f -> p s f", p=128)), writes=[Bx])
            if first:
                hsrc = d["xhalo"] if blk == 0 else d["xs"][blk * NT - 16:blk * NT, :]
                k.dma("sp", lambda h, hsrc=hsrc: h.dma_start(out=xh[:], in_=hsrc), writes=[Bxh])
                for q in range(4):
                    tp, TP = tps[q % 2], TPS[q % 2]
                    for j in range(4):
                        dc = q * 4 + j
                        k.op("pe", lambda h, dc=dc, j=j, tp=tp: h.transpose(tp[:, j, 0:16], xh[:, dc * 128:(dc + 1) * 128], c["ident_f"][0:16, 0:16]),
                             reads=[Bxh, CB["ident_f"]], writes=[TP])
                    k.op("act", lambda h, q=q, tp=tp: h.activation(out=xTh[:, q * 4:q * 4 + 4, :], in_=tp[:, :, 0:16], func=AF.Copy), reads=[TP], writes=[BxTh])
            else:
                k.op("dve", lambda h: h.tensor_copy(out=A_t[:, :, 0:16], in_=A_t[:, :, TT:TT + 16]), reads=[BA], writes=[BA])
                k.op("dve", lambda h: h.tensor_copy(out=Z_t[:, :, 0:16], in_=Z_t[:, :, TT:TT + 16]), reads=[BZ], writes=[BZ])
            for s_ in range(2):
                for q in range(4):
                    tp, TP = tps[q % 2], TPS[q % 2]
                    for j in range(4):
                        dc = q * 4 + j
                        k.op("pe", lambda h, dc=dc, j=j, tp=tp, s_=s_: h.transpose(tp[:, j, :], x_tok[:, s_, dc * 128:(dc + 1) * 128], c["ident_f"][:]),
                             reads=[Bx, CB["ident_f"]], writes=[TP])
                    if q % 2 == 0:
                        k.op("act", lambda h, q=q, tp=tp, s_=s_: h.activation(out=xT[:, q * 4:q * 4 + 4, s_ * 128:(s_ + 1) * 128], in_=tp[:, :, :], func=AF.Copy), reads=[TP], writes=[BxT])
                    else:
                        k.op("dve", lambda h, q=q, tp=tp, s_=s_: h.tensor_copy(out=xT[:, q * 4:q * 4 + 4, s_ * 128:(s_ + 1) * 128], in_=tp[:, :, :]), reads=[TP], writes=[BxT])
            for si, comp in enumerate(slabs):
                wb = nslab_total % 2
                issue_slabs_upto(nslab_total)
                nslab_total += 1
                for fc in range(4):
                    hp, HP = hps[fc], HPS[fc]
                    for dc in range(16):
                        k.op("pe", lambda h, hp=hp, wb=wb, dc=dc, fc=fc: h.matmul(hp, lhsT=wslab[wb][:, dc, fc * 128:(fc + 1) * 128], rhs=xT[:, dc, :], start=(dc == 0), stop=(dc == 15)),
                             reads=[BWS[wb], BxT], writes=[HP], inc=(dc == 15))
                    need_halo = first and (si < 2 or si >= 4)
                    if need_halo:
                        hh = hhps[:, fc % 2, :]
                        for dc in range(16):
                            k.op("pe", lambda h, hh=hh, wb=wb, dc=dc, fc=fc: h.matmul(hh, lhsT=wslab[wb][:, dc, fc * 128:(fc + 1) * 128], rhs=xTh[:, dc, :], start=(dc == 0), stop=(dc == 15)),
                                 reads=[BWS[wb], BxTh], writes=[HHPS], inc=(dc == 15))
                    if si < 2:
                        mch = si * 4 + fc
                        k.op("act", lambda h, hp=hp, mch=mch: h.activation(out=A_t[:, mch, 16:16 + TT], in_=hp, func=AF.Copy), reads=[HP], writes=[BA])
                        if need_halo:
                            k.op("act", lambda h, hh=hh, mch=mch: h.activation(out=A_t[:, mch, 0:16], in_=hh, func=AF.Copy), reads=[HHPS], writes=[BA])
                    elif si < 4:
                        mch = (si - 2) * 4 + fc
                        k.op("act", lambda h, hp=hp, mch=mch: h.activation(out=GB[:, mch, :], in_=hp, func=AF.Copy), reads=[HP], writes=[BGB])
                    else:
                        cs = si - 4
                        if fc < 2:
                            k.op("act", lambda h, hp=hp, fc=fc: h.activation(out=GC[:, fc, :], in_=hp, func=AF.Copy), reads=[HP], writes=[BGC])
                            if need_halo:
                                k.op("act", lambda h, hh=hh, fc=fc: h.activation(out=GCh[:, fc, :], in_=hh, func=AF.Copy), reads=[HHPS], writes=[BGCh])
                        else:
                            mch = cs * 2 + (fc - 2)
                            k.op("dve", lambda h, hp=hp, mch=mch, fc=fc: h.tensor_tensor(out=Z_t[:, mch, 16:16 + TT], in0=GC[:, fc - 2, :], in1=hp, op=ALU.mult), reads=[HP, BGC], writes=[BZ])
                            if need_halo:
                                k.op("dve", lambda h, hh=hh, mch=mch, fc=fc: h.tensor_tensor(out=Z_t[:, mch, 0:16], in0=GCh[:, fc - 2, :], in1=hh, op=ALU.mult), reads=[HHPS, BGCh], writes=[BZ])
            for g, w in enumerate((2, 4, 8, 16)):
                steps = {2: 1, 4: 2, 8: 3, 16: 4}[w]
                hi = 16 + TT
                src = lambda lo, hi_, g=g: A_t[:, 2 * g:2 * g + 2, lo:hi_]
                src_lo = 0
                Bsrc = BA
                bufs = [(sA, BsA), (sB, BsB)]
                for Ls in range(1, steps + 1):
                    hlf = 2 ** (Ls - 1)
                    lo = 16 - (w - 2 ** Ls)
                    dst, Bdst = bufs[(Ls - 1) % 2]
                    n = hi - lo
                    a0 = src(lo - src_lo, hi - src_lo)
                    a1 = src(lo - hlf - src_lo, hi - hlf - src_lo)
                    k.op("dve", lambda h, dst=dst, n=n, a0=a0, a1=a1: h.tensor_tensor(out=dst[:, :, 0:n], in0=a0, in1=a1, op=ALU.add), reads=[Bsrc], writes=[Bdst])
                    src = lambda lo_, hi_, dst=dst: dst[:, :, lo_:hi_]
                    src_lo = lo
                    Bsrc = Bdst
                S = src(0, TT)
                k.op("dve", lambda h, S=S, g=g, w=w: h.scalar_tensor_tensor(out=pT[:, 2 * g:2 * g + 2, :], in0=S, scalar=1.0 / w, in1=A_t[:, 2 * g:2 * g + 2, 16:16 + TT], op0=ALU.mult, op1=ALU.subtract),
                     reads=[Bsrc, BA], writes=[BpT])
                if first:
                    Sd = src
                    for cc in range(2):
                        S16 = Sd(0, 16)[:, cc, :]
                        k.op("dve", lambda h, S16=S16, g=g, ch=ch: h.tensor_tensor(out=t16[:], in0=S16, in1=invc[:, ch, g, :], op=ALU.mult), reads=[Bsrc, Bw], writes=[Bt16])
                        k.op("dve", lambda h, g=g, cc=cc: h.tensor_tensor(out=pT[:, 2 * g + cc, 0:16], in0=t16[:], in1=A_t[:, 2 * g + cc, 16:32], op=ALU.subtract), reads=[Bt16, BA], writes=[BpT])
            for mo in range(8):
                g = mo // 2
                pp, PP = pps2[mo % 2], PPS2[mo % 2]
                for cc in range(2):
                    k.op("pe", lambda h, pp=pp, g=g, cc=cc, mo=mo: h.matmul(pp, lhsT=poolw[:, g, cc, (mo % 2) * 128:(mo % 2 + 1) * 128], rhs=pT[:, 2 * g + cc, :], start=(cc == 0), stop=(cc == 1)),
                         reads=[Bw, BpT], writes=[PP], inc=(cc == 1))
                k.op("act", lambda h, pp=pp, mo=mo: h.activation(out=yT[:, mo, :], in_=pp, func=AF.Copy, scale=pscale[:, mo:mo + 1]), reads=[PP, Bw], writes=[ByT])
            for mo in range(8):
                t1, T1 = ct[mo % 2], Bct[mo % 2]
                k.op("dve", lambda h, t1=t1, mo=mo: h.tensor_scalar(out=t1[:], in0=Z_t[:, mo, 16:16 + TT], scalar1=cw[:, 2, mo:mo + 1], scalar2=None, op0=ALU.mult), reads=[BZ, Bw], writes=[T1])
                k.op("dve", lambda h, t1=t1, mo=mo: h.scalar_tensor_tensor(out=t1[:], in0=Z_t[:, mo, 15:15 + TT], scalar=cw[:, 1, mo:mo + 1], in1=t1[:], op0=ALU.mult, op1=ALU.add), reads=[BZ, T1], writes=[T1])
                k.op("dve", lambda h, t1=t1, mo=mo: h.scalar_tensor_tensor(out=t1[:], in0=Z_t[:, mo, 14:14 + TT], scalar=cw[:, 0, mo:mo + 1], in1=t1[:], op0=ALU.mult, op1=ALU.add), reads=[BZ, T1], writes=[T1])
                k.op("dve", lambda h, t1=t1, mo=mo: h.tensor_tensor(out=yT[:, 8 + mo, :], in0=t1[:], in1=GB[:, mo, :], op=ALU.mult), reads=[T1, BGB], writes=[ByT])
            issue_slabs_upto(nslab_total + 1)
            for s_ in range(2):
                for n in range(4):
                    op_, OP = ops[n % 2], OPS[n % 2]
                    for ec in range(16):
                        k.op("pe", lambda h, op_=op_, ec=ec, s_=s_, n=n: h.matmul(op_[:], lhsT=yT[:, ec, s_ * 128:(s_ + 1) * 128], rhs=wout[:, ec, n * 512:(n + 1) * 512], start=(ec == 0), stop=(ec == 15)),
                             reads=[ByT, Bw], writes=[OP], inc=(ec == 15))
                    k.op("dve", lambda h, op_=op_, s_=s_, n=n: h.scalar_tensor_tensor(out=x_tok[:, s_, n * 512:(n + 1) * 512], in0=x_tok[:, s_, n * 512:(n + 1) * 512], scalar=ALPHA, in1=op_[:], op0=ALU.mult, op1=ALU.add),
                         reads=[OP, Bx], writes=[Bx])
                pm.run(x_tok[:, s_, :], Bx, ti * 2 + s_)
        barrier(k)
        k.emit()


def ln_rows(k, c, r, R, sm, st, Bsm, g_t, b_t, Bp):
    CB = c["B"]
    for n in range(4):
        k.op("dve", lambda h, n=n: h.bn_stats(out=st[:, n, :], in_=r[:, n * 512:(n + 1) * 512]), reads=[R], writes=[Bsm])
    k.op("dve", lambda h: h.bn_aggr(out=sm[:, 0:2], in_=st[:, :, :]), reads=[Bsm], writes=[Bsm])
    k.op("act", lambda h: h.activation(out=sm[:, 6:7], in_=sm[:, 1:2], func=AF.Ln, bias=c["eps"][:], scale=1.0), reads=[Bsm, CB["eps"]], writes=[Bsm])
    k.op("act", lambda h: h.activation(out=sm[:, 2:3], in_=sm[:, 6:7], func=AF.Exp, scale=-0.5), reads=[Bsm], writes=[Bsm])
    k.op("dve", lambda h: h.tensor_scalar(out=r, in0=r, scalar1=sm[:, 0:1], scalar2=sm[:, 2:3], op0=ALU.subtract, op1=ALU.mult), reads=[R, Bsm], writes=[R])
    k.op("dve", lambda h: h.tensor_tensor(out=r, in0=r, in1=g_t[:], op=ALU.mult), reads=[R, Bp], writes=[R])
    k.op("dve", lambda h: h.tensor_tensor(out=r, in0=r, in1=b_t[:], op=ALU.add), reads=[R, Bp], writes=[R])


def moe_phase(m, layer, out_rows):
    nc, k, d, c = m.nc, m.k, m.d, m.c
    CB = c["B"]
    L = layer
    nexp = m.nexp
    with ExitStack() as es:
        sb, ps = alloc_fns(nc, es)
        slab = [sb("slab%d" % i, [128, 16, 1024], BF16) for i in range(2)]
        xgT = [sb("xgT%d" % i, [128, 16, CAP], BF16) for i in range(2)]
        xg = [sb("xg%d" % i, [128, D], BF16) for i in range(CQ)]
        hidT = sb("hidT", [128, 16, CAP], BF16)
        tg = [sb("tg%d" % i, [128, SG], F32) for i in range(2)]
        ts_ = [sb("ts%d" % i, [128, SG], F32) for i in range(2)]
        tl = [sb("tl%d" % i, [128, SG], F32) for i in range(2)]
        ystage = [sb("ystage%d" % i, [128, 512], F32) for i in range(3)]
        lists = sb("lists", [128, NE, CQ * 2], I32)
        bstage = sb("bstage", [128, 8, 128], F32)
        bguT = sb("bguT", [128, 1024], F32)
        bdn = [sb("bdn%d" % i, [1, D], BF16) for i in range(2)]
        mbank = [ps("mbank%d" % i, [128, 512], F32) for i in range(6)]
        tbank = [ps("tbank%d" % i, [128, 1024], BF16) for i in range(2)]
        MB = [Buf() for _ in range(6)]; TBK = [Buf(), Buf()]
        BSL = [Buf(), Buf()]; BXGT = [Buf(), Buf()]; BXG = [Buf() for _ in range(CQ)]; BH = Buf()
        BTG = [Buf(), Buf()]; BTS = [Buf(), Buf()]; BTL = [Buf(), Buf()]; BYS = [Buf() for _ in range(3)]
        BLI = Buf(); BBS = Buf(); BBG = Buf(); BBD = [Buf(), Buf()]
        k.dma("sp", lambda h: h.dma_start(out=lists[:], in_=d["tok_list"].rearrange("(e p q) o -> p e (q o)", p=128, q=CQ)), reads=[m.Btoklist], writes=[BLI])
        k.dma("sp", lambda h: h.dma_start(out=bstage[:], in_=d["b_gu"][L].rearrange("e (c f) -> (e c) f", f=128).rearrange("(rr p) f -> p rr f", p=128)), writes=[BBS])
        for rr in range(8):
            bk = 4 + rr // 4
            k.op("pe", lambda h, rr=rr, bk=bk: h.transpose(mbank[bk][:, (rr % 4) * 128:(rr % 4 + 1) * 128], bstage[:, rr, :], c["ident_f"][:]), reads=[BBS, CB["ident_f"]], writes=[MB[bk]])
            if rr % 4 == 3:
                k.op("dve", lambda h, rr=rr, bk=bk: h.tensor_copy(out=bguT[:, (rr - 3) * 128:(rr + 1) * 128], in_=mbank[bk][:, :]), reads=[MB[bk]], writes=[BBG])
        wgu = d["w_gu"]; wdn = d["w_down"]
        slab_list = []
        for e in range(nexp):
            for j in range(4):
                slab_list.append(("gu", e, j))
            for j2 in range(2):
                slab_list.append(("dn", e, j2))
        state = {"next": 0}

        def issue_slab():
            n = state["next"]
            if n >= len(slab_list):
                return
            state["next"] = n + 1
            kind, e, j = slab_list[n]
            bi = n % 2
            if kind == "gu":
                v = wgu[L, e].rearrange("(c p) f -> p c f", p=128)
                k.dma("pool", lambda h, v=v, bi=bi, j=j: h.dma_start(out=slab[bi][:, :, 0:512], in_=v[:, :, j * 512:(j + 1) * 512]), writes=[BSL[bi]])
                k.dma("pool", lambda h, v=v, bi=bi, j=j: h.dma_start(out=slab[bi][:, :, 512:1024], in_=v[:, :, 2048 + j * 512:2048 + (j + 1) * 512]), writes=[BSL[bi]])
            else:
                v = wdn[L, e].rearrange("(c p) f -> p c f", p=128)
                k.dma("pool", lambda h, v=v, bi=bi, j=j: h.dma_start(out=slab[bi][:, :, :], in_=v[:, :, j * 1024:(j + 1) * 1024]), writes=[BSL[bi]])

        def gathers(e):
            xi = e % 2
            k.dma("pool", lambda h, e=e, xi=xi: h.dma_start(out=bdn[xi][:], in_=d["b_down"][L, e:e + 1, :]), writes=[BBD[xi]])
            for q in range(CQ):
                gi = q
                k.dma("pool", lambda h, e=e, q=q, gi=gi: h.indirect_dma_start(out=xg[gi][:], out_offset=None, in_=d["xm_bf"],
                      in_offset=bass.IndirectOffsetOnAxis(ap=lists[:, e, 2 * q:2 * q + 1], axis=0)), reads=[BLI, m.Bxmb], writes=[BXG[gi]])

        def gather_transpose(e):
            xi = e % 2
            for q in range(CQ):
                gi = q
                for hf in range(2):
                    tb, TB = tbank[hf], TBK[hf]
                    for j in range(8):
                        dc = hf * 8 + j
                        k.op("pe", lambda h, tb=tb, j=j, dc=dc, gi=gi: h.transpose(tb[:, j * 128:(j + 1) * 128], xg[gi][:, dc * 128:(dc + 1) * 128], c["ident_bf"][:]),
                             reads=[BXG[gi], CB["ident_bf"]], writes=[TB])
                    src = tb[:, :].rearrange("p (j t) -> p j t", j=8)
                    dst = xgT[xi][:, hf * 8:hf * 8 + 8, q * 128:(q + 1) * 128]
                    if hf == 0:
                        k.op("act", lambda h, src=src, dst=dst: h.activation(out=dst, in_=src, func=AF.Copy), reads=[TB], writes=[BXGT[xi]])
                    else:
                        k.op("dve", lambda h, src=src, dst=dst: h.tensor_copy(out=dst, in_=src), reads=[TB], writes=[BXGT[xi]])

        unit = {"n": 0, "y": 0}

        def mm1(e, bi, j):
            xi = e % 2
            for fc in range(4):
                F = j * 4 + fc
                for sg in range(2):
                    u = unit["n"]; unit["n"] += 1
                    gb, GBk = mbank[(u % 2) * 2], MB[(u % 2) * 2]
                    lb, LBk = mbank[(u % 2) * 2 + 1], MB[(u % 2) * 2 + 1]
                    tgi, tsi, tli = tg[u % 2], ts_[u % 2], tl[u % 2]
                    BG, BS_, BL_ = BTG[u % 2], BTS[u % 2], BTL[u % 2]
                    for dc in range(16):
                        k.op("pe", lambda h, gb=gb, bi=bi, dc=dc, fc=fc, xi=xi, sg=sg: h.matmul(gb[:, 0:SG], lhsT=slab[bi][:, dc, fc * 128:(fc + 1) * 128], rhs=xgT[xi][:, dc, sg * SG:(sg + 1) * SG], start=(dc == 0), stop=(dc == 15)),
                             reads=[BSL[bi], BXGT[xi]], writes=[GBk], inc=(dc == 15))
                    for dc in range(16):
                        k.op("pe", lambda h, lb=lb, bi=bi, dc=dc, fc=fc, xi=xi, sg=sg: h.matmul(lb[:, 0:SG], lhsT=slab[bi][:, dc, 512 + fc * 128:512 + (fc + 1) * 128], rhs=xgT[xi][:, dc, sg * SG:(sg + 1) * SG], start=(dc == 0), stop=(dc == 15)),
                             reads=[BSL[bi], BXGT[xi]], writes=[LBk], inc=(dc == 15))
                    cg = e * 32 + F
                    cl = e * 32 + 16 + F
                    k.op("dve", lambda h, gb=gb, tgi=tgi, cg=cg: h.tensor_scalar(out=tgi[:], in0=gb[:, 0:SG], scalar1=bguT[:, cg:cg + 1], scalar2=7.0, op0=ALU.add, op1=ALU.min), reads=[GBk, BBG], writes=[BG])
                    k.op("act", lambda h, tgi=tgi, tsi=tsi: h.activation(out=tsi[:], in_=tgi[:], func=AF.Sigmoid, scale=1.702), reads=[BG], writes=[BS_])
                    k.op("act", lambda h, lb=lb, tli=tli, cl=cl: h.activation(out=tli[:], in_=lb[:, 0:SG], func=AF.Identity, bias=bguT[:, cl:cl + 1], scale=1.0), reads=[LBk, BBG], writes=[BL_])
                    k.op("dve", lambda h, tli=tli: h.tensor_scalar(out=tli[:], in0=tli[:], scalar1=-7.0, scalar2=7.0, op0=ALU.max, op1=ALU.min), reads=[BL_], writes=[BL_])
                    k.op("dve", lambda h, tgi=tgi, tsi=tsi: h.tensor_tensor(out=tgi[:], in0=tgi[:], in1=tsi[:], op=ALU.mult), reads=[BG, BS_], writes=[BG])
                    k.op("dve", lambda h, tgi=tgi, tli=tli, F=F, sg=sg: h.scalar_tensor_tensor(out=hidT[:, F, sg * SG:(sg + 1) * SG], in0=tli[:], scalar=1.0, in1=tgi[:], op0=ALU.add, op1=ALU.mult), reads=[BG, BL_], writes=[BH])

        def mm2(e, bi, j2):
            xi = e % 2
            for dn in range(2):
                d0 = j2 * 1024 + dn * 512
                for q in range(CQ):
                    u = unit["y"]; unit["y"] += 1
                    yb, YB = mbank[4 + u % 2], MB[4 + u % 2]
                    ys, YS = ystage[u % 3], BYS[u % 3]
                    for F in range(16):
                        k.op("pe", lambda h, yb=yb, F=F, q=q, bi=bi, dn=dn: h.matmul(yb[:, :], lhsT=hidT[:, F, q * 128:(q + 1) * 128], rhs=slab[bi][:, F, dn * 512:(dn + 1) * 512], start=(F == 0), stop=False),
                             reads=[BH, BSL[bi]], writes=[YB], inc=False)
                    k.op("pe", lambda h, yb=yb, xi=xi, d0=d0: h.matmul(yb[:, :], lhsT=c["ones_bf"][0:1, 0:128], rhs=bdn[xi][0:1, d0:d0 + 512], start=False, stop=True),
                         reads=[CB["ones_bf"], BBD[xi]], writes=[YB])
                    k.op("act", lambda h, yb=yb, ys=ys: h.activation(out=ys[:], in_=yb[:, :], func=AF.Copy), reads=[YB], writes=[YS])
                    rows = d["ybuf"][e * CAP:(e + 1) * CAP, :].rearrange("(p q) f -> p q f", q=CQ)
                    k.dma("sp", lambda h, rows=rows, q=q, d0=d0, ys=ys: h.dma_start(out=rows[:, q, d0:d0 + 512], in_=ys[:]), reads=[YS], writes=[Buf()])

        issue_slab(); issue_slab()
        gathers(0); gather_transpose(0)
        for e in range(nexp):
            n0 = e * 6
            if e + 1 < nexp:
                gathers(e + 1)
            for j in range(4):
                mm1(e, (n0 + j) % 2, j)
                issue_slab()
            if e + 1 < nexp:
                gather_transpose(e + 1)
            for j2 in range(2):
                mm2(e, (n0 + 4 + j2) % 2, j2)
                issue_slab()
        barrier(k)
        k.emit()
    with ExitStack() as es:
        sb, ps = alloc_fns(nc, es)
        acc = [sb("acc%d" % i, [128, D], F32) for i in range(2)]
        yk = [sb("yk%d" % i, [128, D], F32) for i in range(4)]
        lng = sb("ln2g", [128, D], F32); lnb = sb("ln2b", [128, D], F32)
        sm = sb("cb_sm", [128, 16], F32); st = sb("cb_st", [128, 4, 6], F32)
        BA = [Buf(), Buf()]; BY = [Buf() for _ in range(4)]; Bp = Buf(); Bsm = Buf()
        k.dma("sp", lambda h: h.dma_start(out=lng[:], in_=d["ln2_g"][L:L + 1, :].partition_broadcast(128)), writes=[Bp])
        k.dma("sp", lambda h: h.dma_start(out=lnb[:], in_=d["ln2_b"][L:L + 1, :].partition_broadcast(128)), writes=[Bp])
        for sub in range(m.nsub_combine):
            a, A = acc[sub % 2], BA[sub % 2]
            l0 = sub * 128
            k.dma("sp", lambda h, a=a, l0=l0: h.dma_start(out=a[:], in_=d["xm_f32"][l0:l0 + 128, :]), reads=[m.Bxmf], writes=[A])
            for kk in range(4):
                y, Y = yk[kk], BY[kk]
                k.dma("pool", lambda h, y=y, sub=sub, kk=kk: h.indirect_dma_start(out=y[:], out_offset=None, in_=d["ybuf"],
                      in_offset=bass.IndirectOffsetOnAxis(ap=c["destg"][:, sub, kk:kk + 1], axis=0)), reads=[CB["destg"], m.Bybuf], writes=[Y])
            k.op("dve", lambda h, a=a: h.tensor_scalar(out=a[:], in0=a[:], scalar1=ALPHA, scalar2=None, op0=ALU.mult), reads=[A], writes=[A])
            for kk in range(4):
                y, Y = yk[kk], BY[kk]
                k.op("dve", lambda h, a=a, y=y, sub=sub, kk=kk: h.scalar_tensor_tensor(out=a[:], in0=y[:], scalar=c["g4"][:, sub, kk:kk + 1], in1=a[:], op0=ALU.mult, op1=ALU.add),
                     reads=[Y, A, CB["g4"]], writes=[A])
            ln_rows(k, c, a[:], A, sm, st, Bsm, lng, lnb, Bp)
            k.dma("sp", lambda h, a=a, l0=l0: h.dma_start(out=out_rows(l0), in_=a[:]), reads=[A], writes=[Buf()])
        barrier(k)
        k.emit()


S_SEQ = 16384
QT = 512
NEG = -30000.0


def phase_C1(m, blk):
    nc, k, d, c = m.nc, m.k, m.d, m.c
    CB = c["B"]
    S = m.S
    with ExitStack() as es:
        sb, ps = alloc_fns(nc, es)
        TT1 = 512
        NS1 = TT1 // 128
        x_tok = sb("c1_x", [128, NS1, D], F32)
        xT = sb("c1_xT", [128, 16, TT1], BF16)
        wslab = [sb("c1_ws%d" % i, [128, 16, 512], BF16) for i in range(2)]
        wf = sb("c1_wf", [128, 16, 16], BF16)
        bf_t = sb("c1_bf", [128, 16], F32)
        one_c = sb("c1_one", [128, 1], F32)
        qst = sb("c1_qst", [128, 16, TT1], BF16)
        kst = sb("c1_kst", [128, 16, TT1], BF16)
        vst = [sb("c1_vst%d" % i, [128, D], BF16) for i in range(NS1)]
        lf = [sb("c1_lf%d" % i, [128, 16], F32) for i in range(NS1)]
        bank = [ps("c1_bank%d" % i, [128, 512], F32) for i in range(8)]
        BK = [Buf() for _ in range(8)]
        tps = [bank[2][:, :].rearrange("p (j t) -> p j t", j=4), bank[3][:, :].rearrange("p (j t) -> p j t", j=4)]
        TPS = [BK[2], BK[3]]
        Bx = Buf(); BxT = Buf(); BWS = [Buf(), Buf()]; Bw = Buf(); Bq = Buf(); Bk_ = Buf(); BV = [Buf() for _ in range(NS1)]; BLF = [Buf() for _ in range(NS1)]
        w_v = d["fox_w_in"].rearrange("(c p) f -> p c f", p=128)
        k.dma("pool", lambda h: h.dma_start(out=wf[:], in_=w_v[:, :, 6144:6160]), writes=[Bw])
        k.dma("sp", lambda h: h.dma_start(out=bf_t[:], in_=d["fox_b_f"][0:1, :].partition_broadcast(128)), writes=[Bw])
        k.op("dve", lambda h: h.memset(one_c[:], 1.0), writes=[Bw])
        nsl = 0
        for ti in range(NT // TT1):
            g0 = blk * NT + ti * TT1
            k.dma("sp", lambda h, g0=g0: h.dma_start(out=x_tok[:], in_=d["x1"][g0:g0 + TT1, :].rearrange("(s p) f -> p s f", p=128)), writes=[Bx])
            for s_ in range(NS1):
                for q in range(4):
                    tp, TP = tps[q % 2], TPS[q % 2]
                    for j in range(4):
                        dc = q * 4 + j
                        k.op("pe", lambda h, dc=dc, j=j, tp=tp, s_=s_: h.transpose(tp[:, j, :], x_tok[:, s_, dc * 128:(dc + 1) * 128], c["ident_f"][:]),
                             reads=[Bx, CB["ident_f"]], writes=[TP])
                    if q % 2 == 0:
                        k.op("act", lambda h, q=q, tp=tp, s_=s_: h.activation(out=xT[:, q * 4:q * 4 + 4, s_ * 128:(s_ + 1) * 128], in_=tp[:, :, :], func=AF.Copy), reads=[TP], writes=[BxT])
                    else:
                        k.op("dve", lambda h, q=q, tp=tp, s_=s_: h.tensor_copy(out=xT[:, q * 4:q * 4 + 4, s_ * 128:(s_ + 1) * 128], in_=tp[:, :, :]), reads=[TP], writes=[BxT])
            for si in range(12):
                wb = nsl % 2
                nsl += 1
                k.dma("pool", lambda h, wb=wb, si=si: h.dma_start(out=wslab[wb][:, :, :], in_=w_v[:, :, si * 512:(si + 1) * 512]), writes=[BWS[wb]])
                if si < 8:
                    for fc in range(4):
                        bk = fc % 2
                        hp = bank[bk][:, :]
                        for dc in range(16):
                            k.op("pe", lambda h, hp=hp, wb=wb, dc=dc, fc=fc: h.matmul(hp, lhsT=wslab[wb][:, dc, fc * 128:(fc + 1) * 128], rhs=xT[:, dc, :], start=(dc == 0), stop=(dc == 15)),
                                 reads=[BWS[wb], BxT], writes=[BK[bk]], inc=(dc == 15))
                        hh = (si % 4) * 4 + fc
                        if si < 4:
                            k.op("act", lambda h, hp=hp, hh=hh: h.activation(out=qst[:, hh, :], in_=hp, func=AF.Copy, scale=float(128 ** -0.5)), reads=[BK[bk]], writes=[Bq])
                        else:
                            k.op("dve", lambda h, hp=hp, hh=hh: h.tensor_copy(out=kst[:, hh, :], in_=hp), reads=[BK[bk]], writes=[Bk_])
                else:
                    n = si - 8
                    for s_ in range(NS1):
                        bk = 4 + s_ % 2
                        for dc in range(16):
                            k.op("pe", lambda h, bk=bk, wb=wb, dc=dc, s_=s_: h.matmul(bank[bk][:, :], lhsT=xT[:, dc, s_ * 128:(s_ + 1) * 128], rhs=wslab[wb][:, dc, :], start=(dc == 0), stop=(dc == 15)),
                                 reads=[BWS[wb], BxT], writes=[BK[bk]], inc=(dc == 15))
                        if s_ % 2 == 0:
                            k.op("act", lambda h, bk=bk, n=n, s_=s_: h.activation(out=vst[s_][:, n * 512:(n + 1) * 512], in_=bank[bk][:, :], func=AF.Copy), reads=[BK[bk]], writes=[BV[s_]])
                        else:
                            k.op("dve", lambda h, bk=bk, n=n, s_=s_: h.tensor_copy(out=vst[s_][:, n * 512:(n + 1) * 512], in_=bank[bk][:, :]), reads=[BK[bk]], writes=[BV[s_]])
            for s_ in range(NS1):
                fp = bank[6][:, s_ * 16:(s_ + 1) * 16]
                for dc in range(16):
                    k.op("pe", lambda h, fp=fp, dc=dc, s_=s_: h.matmul(fp, lhsT=xT[:, dc, s_ * 128:(s_ + 1) * 128], rhs=wf[:, dc, :], start=(dc == 0), stop=(dc == 15)),
                         reads=[Bw, BxT], writes=[BK[6]], inc=(dc == 15))
                l_, LF = lf[s_], BLF[s_]
                k.op("dve", lambda h, fp=fp, l_=l_: h.tensor_tensor(out=l_[:], in0=fp, in1=bf_t[:], op=ALU.add), reads=[BK[6], Bw], writes=[LF])
                k.op("act", lambda h, l_=l_: h.activation(out=l_[:], in_=l_[:], func=AF.Exp, scale=-1.0), reads=[LF], writes=[LF])
                k.op("act", lambda h, l_=l_: h.activation(out=l_[:], in_=l_[:], func=AF.Ln, bias=one_c[:], scale=1.0), reads=[LF, Bw], writes=[LF])
                k.op("dve", lambda h, l_=l_: h.tensor_scalar(out=l_[:], in0=l_[:], scalar1=-1.0, scalar2=None, op0=ALU.mult), reads=[LF], writes=[LF])
                gs = g0 + s_ * 128
                k.dma("sp", lambda h, l_=l_, gs=gs: h.dma_start(out=d["logf"][gs:gs + 128, :], in_=l_[:]), reads=[LF], writes=[Buf()])
                jt = gs // 128
                k.dma("sp", lambda h, s_=s_, jt=jt: h.dma_start(out=d["v_p"][:, :, jt, :].rearrange("h p e -> p h e"), in_=vst[s_][:, :].rearrange("p (h e) -> p h e", e=128)), reads=[BV[s_]], writes=[Buf()])
            k.dma("sp", lambda h, g0=g0: h.dma_start(out=d["qT"][:, :, g0:g0 + TT1].rearrange("h p t -> p h t"), in_=qst[:]), reads=[Bq], writes=[Buf()])
            k.dma("sp", lambda h, g0=g0: h.dma_start(out=d["kT"][:, :, g0:g0 + TT1].rearrange("h p t -> p h t"), in_=kst[:]), reads=[Bk_], writes=[Buf()])
        barrier(k)
        k.emit()


def attn_setup(m):
    nc, k, d, c = m.nc, m.k, m.d, m.c
    CB = c["B"]
    S = m.S
    NKT = S // 128
    with ExitStack() as es:
        sb, ps = alloc_fns(nc, es)
        lfa = sb("as_lfa", [128, NKT, 16], F32)
        negc = sb("as_negc", [128, NKT, 16], F32)
        linc = sb("as_linc", [128, 128], F32)
        onesf = sb("as_ones", [128, 128], F32)
        R = sb("as_R", [128, 16], F32)
        crow = [sb("as_crow%d" % i, [16, 2048], BF16) for i in range(2)]
        pb = [ps("as_pb%d" % i, [128, 512], F32) for i in range(4)]
        PB = [Buf() for _ in range(4)]
        Bl = Buf(); Bn = Buf(); Bc = Buf(); BR = Buf(); BCR = [Buf(), Buf()]
        k.dma("sp", lambda h: h.dma_start(out=lfa[:], in_=d["logf"].rearrange("(j p) h -> p j h", p=128)), writes=[Bl])
        k.dma("sp", lambda h: h.dma_start(out=linc[:], in_=d["lincl"]), writes=[Bc])
        k.op("dve", lambda h: h.memset(onesf[:], 1.0), writes=[Bc])
        k.op("dve", lambda h: h.memset(R[:], 0.0), writes=[BR])
        for j in range(NKT):
            p0, P0 = pb[j % 2], PB[j % 2]
            p1, P1 = pb[2 + j % 2], PB[2 + j % 2]
            k.op("pe", lambda h, p0=p0, j=j: h.matmul(p0[:, 0:16], lhsT=linc[:], rhs=lfa[:, j, :], start=True, stop=False), reads=[Bc, Bl], writes=[P0], inc=False)
            k.op("pe", lambda h, p0=p0: h.matmul(p0[:, 0:16], lhsT=onesf[:], rhs=R[:], start=False, stop=True), reads=[Bc, BR], writes=[P0])
            k.op("pe", lambda h, p1=p1, j=j: h.matmul(p1[0:16, 0:128], lhsT=lfa[:, j, :], rhs=linc[:], start=True, stop=False), reads=[Bc, Bl], writes=[P1], inc=False)
            k.op("pe", lambda h, p1=p1: h.matmul(p1[0:16, 0:128], lhsT=R[:], rhs=onesf[:], start=False, stop=True), reads=[Bc, BR], writes=[P1])
            k.op("dve", lambda h, j=j: h.tensor_tensor(out=R[:], in0=R[:], in1=lfa[:, j, :], op=ALU.add), reads=[Bl, BR], writes=[BR])
            k.op("act", lambda h, p0=p0, j=j: h.activation(out=negc[:, j, :], in_=p0[:, 0:16], func=AF.Copy, scale=-1.0), reads=[P0], writes=[Bn])
            ci = (j // 16) % 2
            k.op("dve", lambda h, p1=p1, j=j, ci=ci: h.tensor_copy(out=crow[ci][:, (j % 16) * 128:(j % 16 + 1) * 128], in_=p1[0:16, 0:128]), reads=[P1], writes=[BCR[ci]])
            if j % 16 == 15:
                j0 = (j // 16) * 2048
                k.dma("sp", lambda h, ci=ci, j0=j0: h.dma_start(out=d["crow_d"][:, j0:j0 + 2048], in_=crow[ci][:]), reads=[BCR[ci]], writes=[Buf()])
        k.dma("sp", lambda h: h.dma_start(out=d["negc_d"], in_=negc[:].rearrange("p j h -> p (j h)")), reads=[Bn], writes=[Buf()])
        barrier(k)
        k.emit()


def phase_attn(m, blk):
    nc, k, d, c = m.nc, m.k, m.d, m.c
    CB = c["B"]
    S = m.S
    NKT = S // 128
    with ExitStack() as es:
        sb, ps = alloc_fns(nc, es)
        negc = sb("at_negc", [128, NKT, 16], F32)
        sel = sb("at_sel", [16, 16, 128], BF16)
        mask = sb("at_mask", [128, 896], BF16)
        crow = [sb("at_crow%d" % i, [16, QT], BF16) for i in range(2)]
        qt = [sb("at_q%d" % i, [128, QT], BF16) for i in range(2)]
        kc = [sb("at_k%d" % i, [128, 2048], BF16) for i in range(3)]
        vc = [sb("at_v%d" % i, [128, 16, 128], BF16) for i in range(3)]
        pt = [sb("at_p%d" % i, [128, QT], BF16) for i in range(3)]
        rc = sb("at_rc", [128, QT], F32)

        oT = sb("at_oT", [128, 16, QT], BF16)
        wo = sb("at_wo", [128, 16, D], BF16)
        rt = [sb("at_r%d" % i, [128, D], F32) for i in range(2)]
        bank = [ps("at_bank%d" % i, [128, 512], F32) for i in range(8)]
        BK = [Buf() for _ in range(8)]
        SB_, OB, LB, OPB = (0, 1), (2, 3), (4, 5), (6, 7)
        tps = [bank[0][:, :].rearrange("p (j t) -> p j t", j=4), bank[1][:, :].rearrange("p (j t) -> p j t", j=4)]
        pm = PostMixer(m, sb, ps, 1, tps, [BK[0], BK[1]], bank[6][:, 32:64], bank[6][:, 64:96], BK[6])
        Bw = Buf(); BCRW = [Buf(), Buf()]; BQ = [Buf(), Buf()]; BKC = [Buf() for _ in range(3)]; BVC = [Buf() for _ in range(3)]
        BPT = [Buf() for _ in range(3)]; BRC = Buf(); BOT = Buf(); BRT = [Buf(), Buf()]
        k.dma("sp", lambda h: h.dma_start(out=negc[:].rearrange("p j h -> p (j h)"), in_=d["negc_d"]), writes=[Bw])
        k.dma("pool", lambda h: h.dma_start(out=sel[:].rearrange("a b c -> a (b c)"), in_=d["sel"]), writes=[Bw])
        k.dma("pool", lambda h: h.dma_start(out=mask[:], in_=d["maskm"]), writes=[Bw])
        k.dma("pool", lambda h: h.dma_start(out=wo[:], in_=d["fox_w_o"].rearrange("(c p) f -> p c f", p=128)), writes=[Bw])
        cnt = {"kv": 0, "s": 0, "p": 0, "qh": 0}
        for Ql in range(NT // QT):
            Q = blk * (NT // QT) + Ql
            T0 = Q * QT
            ci = Ql % 2
            k.dma("sp", lambda h, ci=ci, T0=T0: h.dma_start(out=crow[ci][:], in_=d["crow_d"][:, T0:T0 + QT]), writes=[BCRW[ci]])
            nkt = (T0 + QT) // 128
            for hh in range(16):
                qi = cnt["qh"] % 2
                cnt["qh"] += 1
                k.dma("sp", lambda h, qi=qi, hh=hh, T0=T0: h.dma_start(out=qt[qi][:], in_=d["qT"][hh, :, T0:T0 + QT]), writes=[BQ[qi]])
                ob, OBk = bank[OB[qi]], BK[OB[qi]]
                lb, LBk = bank[LB[qi]], BK[LB[qi]]
                nch = (nkt + 15) // 16
                jobs = []
                for cch in range(nch):
                    for jj in range(min(16, nkt - cch * 16)):
                        jobs.append((cch, jj))
                chunk_buf = {}

                def load_chunk(cch):
                    if cch in chunk_buf or cch >= nch:
                        return
                    kvi = cnt["kv"] % 3
                    cnt["kv"] += 1
                    chunk_buf[cch] = kvi
                    k.dma("sp", lambda h, kvi=kvi, hh=hh, cch=cch: h.dma_start(out=kc[kvi][:], in_=d["kT"][hh, :, cch * 2048:(cch + 1) * 2048]), writes=[BKC[kvi]])
                    k.dma("sp", lambda h, kvi=kvi, hh=hh, cch=cch: h.dma_start(out=vc[kvi][:], in_=d["v_p"][hh, :, cch * 16:(cch + 1) * 16, :]), writes=[BVC[kvi]])

                def emit_S(job):
                    cch, jj = job
                    load_chunk(cch)
                    if jj == 0:
                        load_chunk(cch + 1)
                    kvi = chunk_buf[cch]
                    j = cch * 16 + jj
                    si = cnt["s"] % 2
                    cnt["s"] += 1
                    sbk, SBk = bank[SB_[si]], BK[SB_[si]]
                    off = j * 128 - T0
                    diag = off >= 0
                    k.op("pe", lambda h, sbk=sbk, kvi=kvi, jj=jj, qi=qi: h.matmul(sbk[:, :], lhsT=kc[kvi][:, jj * 128:(jj + 1) * 128], rhs=qt[qi][:], start=True, stop=False),
                         reads=[BKC[kvi], BQ[qi]], writes=[SBk], inc=False)
                    k.op("pe", lambda h, sbk=sbk, hh=hh, ci=ci, diag=diag: h.matmul(sbk[:, :], lhsT=sel[:, hh, :], rhs=crow[ci][:], start=False, stop=(not diag)),
                         reads=[Bw, BCRW[ci]], writes=[SBk], inc=(not diag))
                    if diag:
                        k.op("pe", lambda h, sbk=sbk, off=off: h.matmul(sbk[:, :], lhsT=c["ident_bf"][:], rhs=mask[:, 384 - off:896 - off], start=False, stop=True),
                             reads=[Bw, CB["ident_bf"]], writes=[SBk])
                    return (sbk, SBk, kvi, jj, j)

                def emit_PV(st_, first, last):
                    sbk, SBk, kvi, jj, j = st_
                    pi = cnt["p"] % 3
                    cnt["p"] += 1
                    k.op("act", lambda h, sbk=sbk, pi=pi, j=j, hh=hh: h.activation(out=pt[pi][:], in_=sbk[:, :], func=AF.Exp, bias=negc[:, j, hh:hh + 1], scale=1.0),
                         reads=[SBk, Bw], writes=[BPT[pi]])
                    k.op("pe", lambda h, ob=ob, kvi=kvi, jj=jj, pi=pi, first=first, last=last: h.matmul(ob[:, :], lhsT=vc[kvi][:, jj, :], rhs=pt[pi][:], start=first, stop=last),
                         reads=[BVC[kvi], BPT[pi]], writes=[OBk], inc=last)
                    k.op("pe", lambda h, lb=lb, pi=pi, first=first, last=last: h.matmul(lb[:, :], lhsT=c["ones_bf"][:], rhs=pt[pi][:], start=first, stop=last),
                         reads=[CB["ones_bf"], BPT[pi]], writes=[LBk], inc=True)

                njob = {"n": 0}
                cur = emit_S(jobs[0])
                for i_ in range(len(jobs)):
                    nxt = emit_S(jobs[i_ + 1]) if i_ + 1 < len(jobs) else None
                    emit_PV(cur, i_ == 0, i_ == len(jobs) - 1)
                    cur = nxt
                k.op("dve", lambda h, lb=lb: h.reciprocal(out=rc[:], in_=lb[:, :]), reads=[LBk], writes=[BRC])
                k.op("dve", lambda h, ob=ob, hh=hh: h.tensor_tensor(out=oT[:, hh, :], in0=ob[:, :], in1=rc[:], op=ALU.mult), reads=[OBk, BRC], writes=[BOT])
            for s_ in range(4):
                ri = s_ % 2
                r, R = rt[ri], BRT[ri]
                g0 = T0 + s_ * 128
                k.dma("sp", lambda h, r=r, g0=g0: h.dma_start(out=r[:], in_=d["x1"][g0:g0 + 128, :]), writes=[R])
                for n in range(4):
                    opb, OPk = bank[OPB[n % 2]], BK[OPB[n % 2]]
                    for hh in range(16):
                        k.op("pe", lambda h, opb=opb, hh=hh, s_=s_, n=n: h.matmul(opb[:, :], lhsT=oT[:, hh, s_ * 128:(s_ + 1) * 128], rhs=wo[:, hh, n * 512:(n + 1) * 512], start=(hh == 0), stop=(hh == 15)),
                             reads=[BOT, Bw], writes=[OPk], inc=(hh == 15))
                    k.op("dve", lambda h, opb=opb, r=r, n=n: h.scalar_tensor_tensor(out=r[:, n * 512:(n + 1) * 512], in0=r[:, n * 512:(n + 1) * 512], scalar=ALPHA, in1=opb[:, :], op0=ALU.mult, op1=ALU.add),
                         reads=[OPk, R], writes=[R])
                pm.run(r[:], R, Ql * 4 + s_)
        barrier(k)
        k.emit()


def core_tokens(r):
    b, c = r // 4, r % 4
    return b, [c * 2048, (7 - c) * 2048]

def consts():
    ident = np.eye(128, dtype=np.float32)
    lstrict = np.triu(np.ones((128, 128), np.float32), 1)
    iota32 = np.tile(np.arange(32, dtype=np.float32)[None, :], (128, 1))
    tokid = (np.arange(32, dtype=np.int32)[None, :] * 128 + np.arange(128, dtype=np.int32)[:, None]).astype(np.int32)
    tokid = np.ascontiguousarray(np.repeat(tokid, 2, axis=1))
    return {"ident": ident, "lstrict": lstrict, "iota32": iota32, "tokid": tokid}

def core_inputs_A(x, r):
    b, starts = core_tokens(r)
    xs = np.concatenate([x[b, s:s + 2048] for s in starts], axis=0)
    xhalo = np.zeros((2, 16, 2048), np.float32)
    invc = np.zeros((128, 2, 4, 16), np.float32)
    for ch, s in enumerate(starts):
        if s > 0:
            xhalo[ch] = x[b, s - 16:s]
        for g, w in enumerate((2, 4, 8, 16)):
            t = s + np.arange(16)
            invc[:, ch, g, :] = (1.0 / np.minimum(t + 1, w)).astype(np.float32)[None, :]
    return {"xs": np.ascontiguousarray(xs), "xhalo": xhalo, "invc": invc}

def consts_attn():
    lincl = np.triu(np.ones((128, 128), np.float32), 0)
    sel = np.zeros((16, 16, 128), np.float32)
    for h in range(16):
        sel[h, h, :] = 1.0
    x = np.arange(896)[None, :]; s = np.arange(128)[:, None]
    maskm = np.where((x - 384) < s, -30000.0, 0.0).astype(np.float32)
    return {"lincl": lincl, "sel": sel.reshape(16, 2048), "maskm": maskm}


def build_program():
    nc = bass.Bass("TRN2", target_bir_lowering=False)
    m = MK(nc)
    m.S = S_SEQ
    S = S_SEQ
    d = {}
    for n, sh, dt in [("ident", [128, 128], F32), ("lstrict", [128, 128], F32), ("iota32", [128, 32], F32), ("tokid", [128, 64], I32),
                      ("lincl", [128, 128], F32), ("sel", [16, 2048], F32), ("maskm", [128, 896], F32),
                      ("xs", [S, D], F32), ("xhalo", [16, D], F32), ("invc", [128, 2, 4, 16], F32),
                      ("ab_w_in", [D, 4096], F32), ("ab_pool_w", [4, 256, 256], F32), ("ab_w_out", [D, D], F32),
                      ("pscale_t", [128, 8], F32), ("cw_t", [128, 3, 8], F32),
                      ("fox_w_in", [D, 6160], F32), ("fox_b_f", [1, 16], F32), ("fox_w_o", [D, D], F32),
                      ("ln1_g", [2, D], F32), ("ln1_b", [2, D], F32), ("ln2_g", [2, D], F32), ("ln2_b", [2, D], F32),
                      ("router_w", [2, D, 32], F32), ("router_b", [2, 32], F32),
                      ("w_gu", [2, NE, D, 4096], F32), ("b_gu", [2, NE, 4096], F32), ("w_down", [2, NE, D, D], F32), ("b_down", [2, NE, D], F32)]:
        d[n] = m.dram_in(n, sh, dt)
    d["out"] = m.dram_out("out", [S, D], F32)
    d["xm_f32"] = m.dram_scr("xm_f32", [NT, D], F32)
    d["xm_bf"] = m.dram_scr("xm_bf", [NT, D], BF16)
    d["ybuf"] = m.dram_scr("ybuf", [NE * CAP, D], F32)
    d["tok_list"] = m.dram_scr("tok_list", [NE * CAP, 2], I32)
    d["x1"] = m.dram_scr("x1", [S, D], F32)
    d["qT"] = m.dram_scr("qT", [16, 128, S], BF16)
    d["kT"] = m.dram_scr("kT", [16, 128, S], BF16)
    d["v_p"] = m.dram_scr("v_p", [16, 128, S // 128, 128], BF16)
    d["logf"] = m.dram_scr("logf", [S, 16], F32)
    d["negc_d"] = m.dram_scr("negc_d", [128, (S // 128) * 16], F32)
    d["crow_d"] = m.dram_scr("crow_d", [16, S], BF16)
    m.d = d
    m.Btoklist = Buf(); m.Bxmf = Buf(); m.Bxmb = Buf(); m.Bybuf = Buf(); m.Bout = Buf()
    m.nexp = NE; m.nsub_combine = NSUB
    k = m.k
    nblk = S // NT
    with ExitStack() as es:
        sb, ps = alloc_fns(nc, es)
        m.c = load_consts(m, sb)
        for blk in range(nblk):
            phase_A(m, blk)
            moe_phase(m, 0, lambda l0, blk=blk: d["x1"][blk * NT + l0:blk * NT + l0 + 128, :])
        for blk in range(nblk):
            phase_C1(m, blk)
        attn_setup(m)
        for blk in range(nblk):
            phase_attn(m, blk)
            moe_phase(m, 1, lambda l0, blk=blk: d["out"][blk * NT + l0:blk * NT + l0 + 128, :])
        barrier(k)
        k.emit()
    return nc, m


def kernel(x, ab_w_in, ab_pool_w, ab_pool_scale, ab_conv_w, ab_w_out, fox_w_in, fox_b_f, fox_w_o,
           ln1_g, ln1_b, ln2_g, ln2_b, router_w, router_b, w_gu, b_gu, w_down, b_down):
    from concourse.bass_utils import run_bass_kernel_spmd
    f32 = np.float32
    a = lambda v: np.ascontiguousarray(np.asarray(v, dtype=f32))
    x = a(x)
    nc, m = build_program()
    shared = dict(consts()); shared.update(consts_attn())
    invc = np.zeros((128, 2, 4, 16), f32)
    for g, w in enumerate((2, 4, 8, 16)):
        t = np.arange(16)
        invc[:, 0, g, :] = (1.0 / np.minimum(t + 1, w)).astype(f32)[None, :]
        invc[:, 1, g, :] = f32(1.0 / w)
    shared.update({
        "xhalo": np.zeros((16, D), f32), "invc": invc,
        "ab_w_in": a(ab_w_in)[0], "ab_pool_w": a(ab_pool_w)[0], "ab_w_out": a(ab_w_out)[0],
        "pscale_t": np.ascontiguousarray(a(ab_pool_scale)[0].reshape(8, 128).T),
        "cw_t": np.ascontiguousarray(a(ab_conv_w)[0].reshape(3, 8, 128).transpose(2, 0, 1)),
        "fox_w_in": a(fox_w_in)[0], "fox_b_f": a(fox_b_f), "fox_w_o": a(fox_w_o)[0],
        "ln1_g": a(ln1_g), "ln1_b": a(ln1_b), "ln2_g": a(ln2_g), "ln2_b": a(ln2_b),
        "router_w": a(router_w), "router_b": a(router_b),
        "w_gu": a(w_gu), "b_gu": a(b_gu), "w_down": a(w_down), "b_down": a(b_down),
    })
    in_maps = []
    for b in range(2):
        im = dict(shared)
        im["xs"] = np.ascontiguousarray(x[b])
        in_maps.append(im)
    res = run_bass_kernel_spmd(nc, in_maps, core_ids=[0, 1])
    out = np.stack([np.asarray(res.results[b]["out"], dtype=f32) for b in range(2)], axis=0)
    return out
```

```python
from contextlib import ExitStack
import numpy as np
import concourse.bass as bass
import concourse.mybir as mybir

F32 = mybir.dt.float32
BF16 = mybir.dt.bfloat16
I32 = mybir.dt.int32
U32 = mybir.dt.uint32
ALU = mybir.AluOpType
AF = mybir.ActivationFunctionType
AX = mybir.AxisListType


class Buf:
    __slots__ = ("name", "w", "r")

    def __init__(self, name=""):
        self.name = name
        self.w = None
        self.r = {}


class KB:
    ENG = ("pe", "act", "dve", "pool", "sp")

    def __init__(self, nc, ndma_sems=24):
        self.nc = nc
        self.h = {"pe": nc.tensor, "act": nc.scalar, "dve": nc.vector, "pool": nc.gpsimd, "sp": nc.sync}
        self.prog = {e: [] for e in self.ENG}
        self.sem = {}
        self.cnt = {e: 0 for e in self.ENG}
        self.waited = {e: {} for e in self.ENG}
        self._cm = []
        for e in self.ENG:
            cm = nc.semaphore("s_" + e)
            self.sem[e] = cm.__enter__()
            self._cm.append(cm)
        self.dsem = []
        self.dcnt = []
        for i in range(ndma_sems):
            cm = nc.semaphore("d%d" % i)
            self.dsem.append(cm.__enter__())
            self._cm.append(cm)
            self.dcnt.append(0)
        self.dnext = 0
        self.same_engine_sync = True
        self.n_inst = 0

    def _wait(self, eng, tok):
        if tok is None:
            return
        sem, val, src = tok
        if src == eng and (eng == "pe" or not self.same_engine_sync):
            return
        key = id(sem)
        if self.waited[eng].get(key, 0) >= val:
            return
        self.waited[eng][key] = val
        h = self.h[eng]
        self.prog[eng].append(lambda: h.wait_ge(sem, val))

    def _deps(self, eng, reads, writes):
        for b in reads:
            self._wait(eng, b.w)
        for b in writes:
            self._wait(eng, b.w)
            for t in b.r.values():
                self._wait(eng, t)

    def _commit(self, tok, reads, writes):
        for b in reads:
            o = b.r.get(id(tok[0]))
            if o is None or o[1] < tok[1]:
                b.r[id(tok[0])] = tok
        for b in writes:
            b.w = tok
            b.r = {}

    def op(self, eng, fn, reads=(), writes=(), inc=True):
        self._deps(eng, reads, writes)
        h = self.h[eng]
        sem = self.sem[eng]
        self.n_inst += 1
        if inc:
            self.cnt[eng] += 1
            self.prog[eng].append(lambda: fn(h).then_inc(sem, 1))
            tok = (sem, self.cnt[eng], eng)
        else:
            self.prog[eng].append(lambda: fn(h))
            tok = (sem, self.cnt[eng] + 1, eng)
        self._commit(tok, reads, writes)
        return tok

    def dma(self, eng, fn, reads=(), writes=()):
        self._deps(eng, reads, writes)
        i = self.dnext
        self.dnext = (self.dnext + 1) % len(self.dsem)
        sem = self.dsem[i]
        self.dcnt[i] += 16
        val = self.dcnt[i]
        h = self.h[eng]
        self.n_inst += 1
        self.prog[eng].append(lambda: fn(h).then_inc(sem, 16))
        tok = (sem, val, "dma")
        self._commit(tok, reads, writes)
        return tok

    def pool_const_reg(self, val):
        if not hasattr(self, "_regs"):
            self._regs = {}
        if val not in self._regs:
            holder = {}
            self._regs[val] = holder
            hp = self.h["pool"]
            self.prog["pool"].append(lambda: holder.__setitem__("r", hp.to_reg(val)))
        return self._regs[val]

    def wait_tok(self, eng, tok):
        self._wait(eng, tok)

    def emit(self):
        nc = self.nc
        with nc.Block() as block:
            @block.tensor
            def _(e):
                for f in self.prog["pe"]:
                    f()

            @block.scalar
            def _(e):
                for f in self.prog["act"]:
                    f()

            @block.vector
            def _(e):
                for f in self.prog["dve"]:
                    f()

            @block.gpsimd
            def _(e):
                for f in self.prog["pool"]:
                    f()

            @block.sync
            def _(e):
                for f in self.prog["sp"]:
                    f()
        self.prog = {e: [] for e in self.ENG}

    def close(self):
        for cm in reversed(self._cm):
            cm.__exit__(None, None, None)


D = 2048
NT = 4096
TT = 512
NSUB = NT // 128
NE = 32
CAP = 768
CQ = CAP // 128
SG = CAP // 2
ALPHA = float(4.0 ** 0.25)
LN_EPS = 1e-5
BIG = 1.0e6


class MK:
    def __init__(self, nc):
        self.nc = nc
        self.k = KB(nc, ndma_sems=32)
        self.es = ExitStack()

    def dram_in(self, name, shape, dt):
        return self.nc.dram_tensor(name, list(shape), dt, kind="ExternalInput").ap()

    def dram_out(self, name, shape, dt):
        return self.nc.dram_tensor(name, list(shape), dt, kind="ExternalOutput").ap()

    def dram_scr(self, name, shape, dt):
        return self.nc.dram_tensor(name, list(shape), dt, kind="Internal").ap()


def alloc_fns(nc, es):
    def sb(name, shape, dt):
        _UID[0] += 1
        return es.enter_context(nc.sbuf_tensor("s%d_%s" % (_UID[0], name), list(shape), dt))

    def ps(name, shape, dt):
        _UID[0] += 1
        return es.enter_context(nc.psum_tensor("p%d_%s" % (_UID[0], name), list(shape), dt))
    return sb, ps


_UID = [0]


def barrier(k):
    for e in k.ENG:
        for e2 in k.ENG:
            if e2 != e and k.cnt[e2]:
                k.wait_tok(e, (k.sem[e2], k.cnt[e2], e2))
        for i, s in enumerate(k.dsem):
            if k.dcnt[i]:
                k.wait_tok(e, (s, k.dcnt[i], "dma"))


def load_consts(m, sb):
    nc, k = m.nc, m.k
    c = {}
    c["ident_f"] = sb("ident_f", [128, 128], F32)
    c["ident_bf"] = sb("ident_bf", [128, 128], BF16)
    c["lstrict"] = sb("lstrict", [128, 128], BF16)
    c["ones_bf"] = sb("ones_bf", [128, 128], BF16)
    c["iota32"] = sb("iota32", [128, 32], F32)
    c["tokid"] = sb("tokid", [128, NSUB, 2], I32)
    c["zero_i"] = sb("zero_i", [128, NE * CQ * 2], I32)
    c["destg"] = sb("destg", [128, NSUB, 4], I32)
    c["g4"] = sb("g4", [128, NSUB, 4], F32)
    c["cmask"] = sb("cmask", [128, 32], BF16)
    c["eps"] = sb("eps", [128, 1], F32)
    B = {n: Buf(n) for n in c}
    c["B"] = B
    d = m.d
    k.dma("sp", lambda h: h.dma_start(out=c["ident_f"][:], in_=d["ident"]), writes=[B["ident_f"]])
    k.dma("pool", lambda h: h.dma_start(out=c["ident_bf"][:], in_=d["ident"]), writes=[B["ident_bf"]])
    k.dma("pool", lambda h: h.dma_start(out=c["lstrict"][:], in_=d["lstrict"]), writes=[B["lstrict"]])
    k.dma("sp", lambda h: h.dma_start(out=c["iota32"][:], in_=d["iota32"]), writes=[B["iota32"]])
    k.dma("sp", lambda h: h.dma_start(out=c["tokid"][:], in_=d["tokid"].rearrange("p (a b) -> p a b", b=2)), writes=[B["tokid"]])
    k.op("dve", lambda h: h.memset(c["ones_bf"][:], 1.0), writes=[B["ones_bf"]])
    k.op("dve", lambda h: h.memset(c["zero_i"][:], 0), writes=[B["zero_i"]])
    k.op("dve", lambda h: h.memset(c["eps"][:], LN_EPS), writes=[B["eps"]])
    return c


class PostMixer:
    def __init__(self, m, sb, ps, layer, tps, TPS, lps, pps, MISC):
        self.m = m
        nc, k, d = m.nc, m.k, m.d
        self.layer = layer
        self.tps, self.TPS = tps, TPS
        self.lng = sb("lng", [128, D], F32)
        self.lnb = sb("lnb", [128, D], F32)
        self.rw = sb("rw", [128, 16, 32], F32)
        self.rb = sb("rb", [128, 32], F32)
        self.xmT = sb("xmT", [128, 16, 128], F32)
        self.st = sb("pm_st", [128, 4, 6], F32)
        self.sm = sb("pm_sm", [128, 96], F32)
        self.smi = sb("pm_smi", [128, 16], U32)
        self.dsc = sb("pm_dsc", [128, 4], I32)
        self.mask = sb("pm_mask", [128, 32], BF16)
        self.oh = sb("pm_oh", [128, 32], F32)
        self.lps = lps
        self.pps = pps
        self.Bp = Buf("lnparams")
        self.BxmT = Buf(); self.Bxmbf = Buf(); self.Bsm = Buf(); self.Blps = MISC; self.Bpps = MISC
        L = layer
        k.dma("sp", lambda h: h.dma_start(out=self.lng[:], in_=d["ln1_g"][L:L + 1, :].partition_broadcast(128)), writes=[self.Bp])
        k.dma("sp", lambda h: h.dma_start(out=self.lnb[:], in_=d["ln1_b"][L:L + 1, :].partition_broadcast(128)), writes=[self.Bp])
        k.dma("sp", lambda h: h.dma_start(out=self.rw[:], in_=d["router_w"][L].rearrange("(c p) e -> p c e", p=128)), writes=[self.Bp])
        k.dma("sp", lambda h: h.dma_start(out=self.rb[:], in_=d["router_b"][L:L + 1, :].partition_broadcast(128)), writes=[self.Bp])
        c = m.c
        k.op("dve", lambda h: h.memset(c["cmask"][:], 0.0), writes=[c["B"]["cmask"]])
        k.dma("sp", lambda h: h.dma_start(out=d["tok_list"].rearrange("(p x) o -> p (x o)", p=128), in_=c["zero_i"][:]),
              reads=[c["B"]["zero_i"]], writes=[m.Btoklist])

    def run(self, r, R, sub):
        m = self.m
        nc, k, d, c = m.nc, m.k, m.d, m.c
        CB = c["B"]
        sm, st = self.sm, self.st
        Bsm = self.Bsm
        l0 = sub * 128
        for n in range(4):
            k.op("dve", lambda h, n=n: h.bn_stats(out=st[:, n, :], in_=r[:, n * 512:(n + 1) * 512]), reads=[R], writes=[Bsm])
        k.op("dve", lambda h: h.bn_aggr(out=sm[:, 0:2], in_=st[:, :, :]), reads=[Bsm], writes=[Bsm])
        k.op("act", lambda h: h.activation(out=sm[:, 6:7], in_=sm[:, 1:2], func=AF.Ln, bias=c["eps"][:], scale=1.0), reads=[Bsm, CB["eps"]], writes=[Bsm])
        k.op("act", lambda h: h.activation(out=sm[:, 2:3], in_=sm[:, 6:7], func=AF.Exp, scale=-0.5), reads=[Bsm], writes=[Bsm])
        k.op("dve", lambda h: h.tensor_scalar(out=r, in0=r, scalar1=sm[:, 0:1], scalar2=sm[:, 2:3], op0=ALU.subtract, op1=ALU.mult), reads=[R, Bsm], writes=[R])
        k.op("dve", lambda h: h.tensor_tensor(out=r, in0=r, in1=self.lng[:], op=ALU.mult), reads=[R, self.Bp], writes=[R])
        k.op("dve", lambda h: h.tensor_tensor(out=r, in0=r, in1=self.lnb[:], op=ALU.add), reads=[R, self.Bp], writes=[R])
        k.dma("sp", lambda h: h.dma_start(out=d["xm_f32"][l0:l0 + 128, :], in_=r), reads=[R], writes=[Buf()])
        k.dma("pool", lambda h: h.dma_start(out=d["xm_bf"][l0:l0 + 128, :], in_=r), reads=[R], writes=[Buf()])
        for q in range(4):
            tp, TP = self.tps[q % 2], self.TPS[q % 2]
            for j in range(4):
                dc = q * 4 + j
                k.op("pe", lambda h, dc=dc, j=j, tp=tp: h.transpose(tp[:, j, :], r[:, dc * 128:(dc + 1) * 128], c["ident_f"][:]),
                     reads=[R, CB["ident_f"]], writes=[TP])
            if q % 2 == 0:
                k.op("act", lambda h, q=q, tp=tp: h.activation(out=self.xmT[:, q * 4:q * 4 + 4, :], in_=tp[:, :, :], func=AF.Copy), reads=[TP], writes=[self.BxmT])
            else:
                k.op("dve", lambda h, q=q, tp=tp: h.tensor_copy(out=self.xmT[:, q * 4:q * 4 + 4, :], in_=tp[:, :, :]), reads=[TP], writes=[self.BxmT])
        for dc in range(16):
            k.op("pe", lambda h, dc=dc: h.matmul(self.lps, lhsT=self.xmT[:, dc, :], rhs=self.rw[:, dc, :], start=(dc == 0), stop=(dc == 15)),
                 reads=[self.BxmT, self.Bp], writes=[self.Blps], inc=(dc == 15))
        lg = sm[:, 8:40]
        k.op("dve", lambda h: h.tensor_tensor(out=lg, in0=self.lps, in1=self.rb[:], op=ALU.add), reads=[self.Blps, self.Bp], writes=[Bsm])
        mx = sm[:, 40:48]
        k.op("dve", lambda h: h.max(out=mx, in_=lg), reads=[Bsm], writes=[Bsm])
        k.op("dve", lambda h: h.max_index(out=self.smi[:, 0:8], in_max=mx, in_values=lg), reads=[Bsm], writes=[Bsm])
        k.op("dve", lambda h: h.tensor_scalar(out=sm[:, 3:4], in0=mx[:, 0:1], scalar1=-1.0, scalar2=None, op0=ALU.mult), reads=[Bsm], writes=[Bsm])
        e4 = sm[:, 48:52]
        k.op("act", lambda h: h.activation(out=e4, in_=mx[:, 0:4], func=AF.Exp, bias=sm[:, 3:4], scale=1.0), reads=[Bsm], writes=[Bsm])
        k.op("dve", lambda h: h.reduce_sum(out=sm[:, 4:5], in_=e4, axis=AX.X), reads=[Bsm], writes=[Bsm])
        k.op("dve", lambda h: h.reciprocal(out=sm[:, 5:6], in_=sm[:, 4:5]), reads=[Bsm], writes=[Bsm])
        g4 = sm[:, 52:56]
        k.op("dve", lambda h: h.tensor_scalar(out=g4, in0=e4, scalar1=sm[:, 5:6], scalar2=None, op0=ALU.mult), reads=[Bsm], writes=[Bsm])
        k.op("dve", lambda h: h.tensor_scalar(out=self.mask[:], in0=lg, scalar1=mx[:, 3:4], scalar2=None, op0=ALU.is_ge), reads=[Bsm], writes=[Bsm])
        k.op("pe", lambda h: h.matmul(self.pps, lhsT=c["lstrict"][:], rhs=self.mask[:], start=True, stop=False),
             reads=[Bsm, CB["lstrict"]], writes=[self.Bpps], inc=False)
        k.op("pe", lambda h: h.matmul(self.pps, lhsT=c["ones_bf"][:], rhs=c["cmask"][:], start=False, stop=True),
             reads=[CB["cmask"], CB["ones_bf"]], writes=[self.Bpps])
        k.op("dve", lambda h: h.tensor_tensor(out=c["cmask"][:], in0=c["cmask"][:], in1=self.mask[:], op=ALU.add), reads=[Bsm], writes=[CB["cmask"]])
        posf = sm[:, 56:88]
        k.op("dve", lambda h: h.tensor_copy(out=posf, in_=self.pps), reads=[self.Bpps], writes=[Bsm])
        idxf = sm[:, 88:92]
        k.op("dve", lambda h: h.tensor_copy(out=idxf, in_=self.smi[:, 0:4]), reads=[Bsm], writes=[Bsm])
        pos4 = sm[:, 92:96]
        for kk in range(4):
            k.op("dve", lambda h, kk=kk: h.tensor_scalar(out=self.oh[:], in0=c["iota32"][:], scalar1=idxf[:, kk:kk + 1], scalar2=None, op0=ALU.is_equal),
                 reads=[Bsm, CB["iota32"]], writes=[Bsm])
            k.op("dve", lambda h: h.tensor_tensor(out=self.oh[:], in0=self.oh[:], in1=posf, op=ALU.mult), reads=[Bsm], writes=[Bsm])
            k.op("dve", lambda h, kk=kk: h.reduce_sum(out=pos4[:, kk:kk + 1], in_=self.oh[:], axis=AX.X), reads=[Bsm], writes=[Bsm])
        valid = sm[:, 6:8]
        valid = self.oh[:, 0:4]
        destf = self.oh[:, 4:8]
        tmpb = self.oh[:, 8:12]
        k.op("dve", lambda h: h.tensor_scalar(out=valid, in0=pos4, scalar1=float(CAP), scalar2=None, op0=ALU.is_lt), reads=[Bsm], writes=[Bsm])
        k.op("dve", lambda h: h.scalar_tensor_tensor(out=destf, in0=idxf, scalar=float(CAP), in1=pos4, op0=ALU.mult, op1=ALU.add), reads=[Bsm], writes=[Bsm])
        k.op("dve", lambda h: h.tensor_tensor(out=c["destg"][:, sub, :], in0=destf, in1=valid, op=ALU.mult), reads=[Bsm], writes=[CB["destg"]])
        k.op("dve", lambda h: h.tensor_tensor(out=c["g4"][:, sub, :], in0=g4, in1=valid, op=ALU.mult), reads=[Bsm], writes=[CB["g4"]])
        k.op("dve", lambda h: h.tensor_scalar(out=tmpb, in0=valid, scalar1=-BIG, scalar2=BIG, op0=ALU.mult, op1=ALU.add), reads=[Bsm], writes=[Bsm])
        k.op("dve", lambda h: h.tensor_tensor(out=self.dsc[:], in0=destf, in1=tmpb, op=ALU.add), reads=[Bsm], writes=[Bsm])
        breg = k.pool_const_reg(NE * CAP - 1)
        for kk in range(4):
            k.dma("pool", lambda h, kk=kk: h.indirect_dma_start(
                out=d["tok_list"], out_offset=bass.IndirectOffsetOnAxis(ap=self.dsc[:, kk:kk + 1], axis=0),
                in_=c["tokid"][:, sub, :], in_offset=None, bounds_check=breg["r"], oob_is_err=False),
                reads=[Bsm, CB["tokid"], m.Btoklist], writes=[Buf()])


def phase_A(m, blk):
    nc, k, d, c = m.nc, m.k, m.d, m.c
    CB = c["B"]
    with ExitStack() as es:
        sb, ps = alloc_fns(nc, es)
        NSA = TT // 128
        x_tok = sb("x_tok", [128, NSA, D], F32)
        xT = sb("xT", [128, 16, TT], BF16)
        xTh = sb("xTh", [128, 16, 16], BF16)
        wslab = [sb("wslab%d" % i, [128, 16, 512], BF16) for i in range(2)]
        A_t = sb("A_t", [128, 8, 16 + TT], F32)
        Z_t = sb("Z_t", [128, 8, 16 + TT], F32)
        GB = sb("GB", [128, 8, TT], F32)
        GC = sb("GC", [128, 2, TT], F32)
        GCh = sb("GCh", [128, 2, 16], F32)
        sA = sb("sA", [128, 2, 16 + TT], F32)
        sB = sb("sB", [128, 2, 16 + TT], F32)
        ct = [sb("ct%d" % i, [128, TT], F32) for i in range(2)]
        t16 = sb("t16", [128, 16], F32)
        pT = sb("pT", [128, 8, TT], BF16)
        yT = sb("yT", [128, 16, TT], BF16)
        poolw = sb("poolw", [128, 4, 2, 256], BF16)
        pscale = sb("pscale", [128, 8], F32)
        cw = sb("cw", [128, 3, 8], F32)
        invc = sb("invc", [128, 2, 4, 16], F32)
        bank = [ps("bank%d" % i, [128, 512], F32) for i in range(8)]
        hps = [bank[0][:, :], bank[1][:, :], bank[0][:, :], bank[1][:, :]]
        tps = [bank[2][:, :].rearrange("p (j t) -> p j t", j=4), bank[3][:, :].rearrange("p (j t) -> p j t", j=4)]
        ops = [bank[4], bank[5]]
        pps2 = [bank[6][:, :], bank[6][:, :]]
        hhps = bank[7][:, 0:32].rearrange("p (j t) -> p j t", j=2)
        TPS = [Buf(), Buf()]
        MISC = Buf("miscbank")
        pm = PostMixer(m, sb, ps, 0, tps, TPS, bank[7][:, 32:64], bank[7][:, 64:96], MISC)
        Bw = Buf("Aweights")
        Bx = Buf("x_tok"); BxT = Buf("xT"); BxTh = Buf()
        xh = pm.xmT[0:16, :, :].rearrange("p a b -> p (a b)")
        Bxh = pm.BxmT
        BWS = [Buf(), Buf()]
        BA = Buf("A_t"); BZ = Buf("Z_t"); BGB = Buf(); BGC = Buf(); BGCh = Buf()
        BsA = Buf(); BsB = Buf(); Bct = [Buf(), Buf()]; Bt16 = Buf()
        BpT = Buf(); ByT = Buf()
        HB0, HB1 = Buf(), Buf()
        HPS = [HB0, HB1, HB0, HB1]; HHPS = MISC; OPS = [Buf(), Buf()]; PB = Buf(); PPS2 = [PB, PB]
        w_out_v = d["ab_w_out"].rearrange("(c p) f -> p c f", p=128)
        k.dma("pool", lambda h: h.dma_start(out=poolw[:], in_=d["ab_pool_w"].rearrange("g (c p) e -> p g c e", p=128)), writes=[Bw])
        k.dma("sp", lambda h: h.dma_start(out=pscale[:], in_=d["pscale_t"]), writes=[Bw])
        k.dma("sp", lambda h: h.dma_start(out=cw[:], in_=d["cw_t"]), writes=[Bw])
        k.dma("sp", lambda h: h.dma_start(out=invc[:], in_=d["invc"]), writes=[Bw])
        k.op("dve", lambda h: h.memset(A_t[:], 0.0), writes=[BA])
        k.op("dve", lambda h: h.memset(Z_t[:], 0.0), writes=[BZ])
        w_in_v = d["ab_w_in"].rearrange("(c p) f -> p c f", p=128)
        slabs = []
        for s_ in range(4):
            slabs.append([(0, s_ * 512, 512, 0)])
        for cs in range(4):
            slabs.append([(0, 2048 + cs * 256, 256, 0), (256, 3072 + cs * 256, 256, 0)])
        for n_ in range(4):
            slabs.append([(0, n_ * 512, 512, 1)])
        nslab_total = 0
        ntiles = NT // TT
        issued = {"n": 0}

        def issue_slabs_upto(nmax):
            while issued["n"] <= nmax and issued["n"] < ntiles * len(slabs):
                n_ = issued["n"]
                issued["n"] += 1
                wb_ = n_ % 2
                for (dc0, sc0, wd, wsel) in slabs[n_ % len(slabs)]:
                    srcv = w_out_v if wsel else w_in_v
                    k.dma("pool", lambda h, wb_=wb_, dc0=dc0, sc0=sc0, wd=wd, srcv=srcv: h.dma_start(out=wslab[wb_][:, :, dc0:dc0 + wd], in_=srcv[:, :, sc0:sc0 + wd]), writes=[BWS[wb_]])
        for ti in range(ntiles):
            ch = 0 if blk == 0 else 1
            first = (ti == 0)
            l0 = blk * NT + ti * TT
            k.dma("sp", lambda h, l0=l0: h.dma_start(out=x_tok[:], in_=d["xs"][l0:l0 + TT, :].rearrange("(s p) f -> p s f", p=128)), writes=[Bx])
            if first:
                hsrc = d["xhalo"] if blk == 0 else d["xs"][blk * NT - 16:blk * NT, :]
                k.dma("sp", lambda h, hsrc=hsrc: h.dma_start(out=xh[:], in_=hsrc), writes=[Bxh])
                for q in range(4):
                    tp, TP = tps[q % 2], TPS[q % 2]
                    for j in range(4):
                        dc = q * 4 + j
                        k.op("pe", lambda h, dc=dc, j=j, tp=tp: h.transpose(tp[:, j, 0:16], xh[:, dc * 128:(dc + 1) * 128], c["ident_f"][0:16, 0:16]),
                             reads=[Bxh, CB["ident_f"]], writes=[TP])
                    k.op("act", lambda h, q=q, tp=tp: h.activation(out=xTh[:, q * 4:q * 4 + 4, :], in_=tp[:, :, 0:16], func=AF.Copy), reads=[TP], writes=[BxTh])
            else:
                k.op("dve", lambda h: h.tensor_copy(out=A_t[:, :, 0:16], in_=A_t[:, :, TT:TT + 16]), reads=[BA], writes=[BA])
                k.op("dve", lambda h: h.tensor_copy(out=Z_t[:, :, 0:16], in_=Z_t[:, :, TT:TT + 16]), reads=[BZ], writes=[BZ])
            for s_ in range(NSA):
                for q in range(4):
                    tp, TP = tps[q % 2], TPS[q % 2]
                    for j in range(4):
                        dc = q * 4 + j
                        k.op("pe", lambda h, dc=dc, j=j, tp=tp, s_=s_: h.transpose(tp[:, j, :], x_tok[:, s_, dc * 128:(dc + 1) * 128], c["ident_f"][:]),
                             reads=[Bx, CB["ident_f"]], writes=[TP])
                    if q % 2 == 0:
                        k.op("act", lambda h, q=q, tp=tp, s_=s_: h.activation(out=xT[:, q * 4:q * 4 + 4, s_ * 128:(s_ + 1) * 128], in_=tp[:, :, :], func=AF.Copy), reads=[TP], writes=[BxT])
                    else:
                        k.op("dve", lambda h, q=q, tp=tp, s_=s_: h.tensor_copy(out=xT[:, q * 4:q * 4 + 4, s_ * 128:(s_ + 1) * 128], in_=tp[:, :, :]), reads=[TP], writes=[BxT])
            def emit_pooling():
                for g, w in enumerate((2, 4, 8, 16)):
                    steps = {2: 1, 4: 2, 8: 3, 16: 4}[w]
                    hi = 16 + TT
                    src = lambda lo, hi_, g=g: A_t[:, 2 * g:2 * g + 2, lo:hi_]
                    src_lo = 0
                    Bsrc = BA
                    bufs = [(sA, BsA), (sB, BsB)]
                    for Ls in range(1, steps + 1):
                        hlf = 2 ** (Ls - 1)
                        lo = 16 - (w - 2 ** Ls)
                        dst, Bdst = bufs[(Ls - 1) % 2]
                        n = hi - lo
                        a0 = src(lo - src_lo, hi - src_lo)
                        a1 = src(lo - hlf - src_lo, hi - hlf - src_lo)
                        k.op("dve", lambda h, dst=dst, n=n, a0=a0, a1=a1: h.tensor_tensor(out=dst[:, :, 0:n], in0=a0, in1=a1, op=ALU.add), reads=[Bsrc], writes=[Bdst])
                        src = lambda lo_, hi_, dst=dst: dst[:, :, lo_:hi_]
                        src_lo = lo
                        Bsrc = Bdst
                    S = src(0, TT)
                    k.op("dve", lambda h, S=S, g=g, w=w: h.scalar_tensor_tensor(out=pT[:, 2 * g:2 * g + 2, :], in0=S, scalar=1.0 / w, in1=A_t[:, 2 * g:2 * g + 2, 16:16 + TT], op0=ALU.mult, op1=ALU.subtract),
                         reads=[Bsrc, BA], writes=[BpT])
                    if first:
                        Sd = src
                        for cc in range(2):
                            S16 = Sd(0, 16)[:, cc, :]
                            k.op("dve", lambda h, S16=S16, g=g, ch=ch: h.tensor_tensor(out=t16[:], in0=S16, in1=invc[:, ch, g, :], op=ALU.mult), reads=[Bsrc, Bw], writes=[Bt16])
                            k.op("dve", lambda h, g=g, cc=cc: h.tensor_tensor(out=pT[:, 2 * g + cc, 0:16], in0=t16[:], in1=A_t[:, 2 * g + cc, 16:32], op=ALU.subtract), reads=[Bt16, BA], writes=[BpT])

            def emit_poolmap():
                for mo in range(8):
                    g = mo // 2
                    pp, PP = pps2[mo % 2], PPS2[mo % 2]
                    for cc in range(2):
                        k.op("pe", lambda h, pp=pp, g=g, cc=cc, mo=mo: h.matmul(pp, lhsT=poolw[:, g, cc, (mo % 2) * 128:(mo % 2 + 1) * 128], rhs=pT[:, 2 * g + cc, :], start=(cc == 0), stop=(cc == 1)),
                             reads=[Bw, BpT], writes=[PP], inc=(cc == 1))
                    k.op("act", lambda h, pp=pp, mo=mo: h.activation(out=yT[:, mo, :], in_=pp, func=AF.Copy, scale=pscale[:, mo:mo + 1]), reads=[PP, Bw], writes=[ByT])

            def emit_conv(conv_mos):
                for mo in conv_mos:
                    t1, T1 = ct[mo % 2], Bct[mo % 2]
                    k.op("dve", lambda h, t1=t1, mo=mo: h.tensor_scalar(out=t1[:], in0=Z_t[:, mo, 16:16 + TT], scalar1=cw[:, 2, mo:mo + 1], scalar2=None, op0=ALU.mult), reads=[BZ, Bw], writes=[T1])
                    k.op("dve", lambda h, t1=t1, mo=mo: h.scalar_tensor_tensor(out=t1[:], in0=Z_t[:, mo, 15:15 + TT], scalar=cw[:, 1, mo:mo + 1], in1=t1[:], op0=ALU.mult, op1=ALU.add), reads=[BZ, T1], writes=[T1])
                    k.op("dve", lambda h, t1=t1, mo=mo: h.scalar_tensor_tensor(out=t1[:], in0=Z_t[:, mo, 14:14 + TT], scalar=cw[:, 0, mo:mo + 1], in1=t1[:], op0=ALU.mult, op1=ALU.add), reads=[BZ, T1], writes=[T1])
                    k.op("dve", lambda h, t1=t1, mo=mo: h.tensor_tensor(out=yT[:, 8 + mo, :], in0=t1[:], in1=GB[:, mo, :], op=ALU.mult), reads=[T1, BGB], writes=[ByT])

            for si, comp in enumerate(slabs[:8]):
                wb = nslab_total % 2
                issue_slabs_upto(nslab_total)
                nslab_total += 1
                for fc in range(4):
                    hp, HP = hps[fc], HPS[fc]
                    for dc in range(16):
                        k.op("pe", lambda h, hp=hp, wb=wb, dc=dc, fc=fc: h.matmul(hp, lhsT=wslab[wb][:, dc, fc * 128:(fc + 1) * 128], rhs=xT[:, dc, :], start=(dc == 0), stop=(dc == 15)),
                             reads=[BWS[wb], BxT], writes=[HP], inc=(dc == 15))
                    need_halo = first and (si < 2 or si >= 4)
                    if need_halo:
                        hh = hhps[:, fc % 2, :]
                        for dc in range(16):
                            k.op("pe", lambda h, hh=hh, wb=wb, dc=dc, fc=fc: h.matmul(hh, lhsT=wslab[wb][:, dc, fc * 128:(fc + 1) * 128], rhs=xTh[:, dc, :], start=(dc == 0), stop=(dc == 15)),
                                 reads=[BWS[wb], BxTh], writes=[HHPS], inc=(dc == 15))
                    if si < 2:
                        mch = si * 4 + fc
                        k.op("act", lambda h, hp=hp, mch=mch: h.activation(out=A_t[:, mch, 16:16 + TT], in_=hp, func=AF.Copy), reads=[HP], writes=[BA])
                        if need_halo:
                            k.op("act", lambda h, hh=hh, mch=mch: h.activation(out=A_t[:, mch, 0:16], in_=hh, func=AF.Copy), reads=[HHPS], writes=[BA])
                    elif si < 4:
                        mch = (si - 2) * 4 + fc
                        k.op("act", lambda h, hp=hp, mch=mch: h.activation(out=GB[:, mch, :], in_=hp, func=AF.Copy), reads=[HP], writes=[BGB])
                    else:
                        cs = si - 4
                        if fc < 2:
                            k.op("act", lambda h, hp=hp, fc=fc: h.activation(out=GC[:, fc, :], in_=hp, func=AF.Copy), reads=[HP], writes=[BGC])
                            if need_halo:
                                k.op("act", lambda h, hh=hh, fc=fc: h.activation(out=GCh[:, fc, :], in_=hh, func=AF.Copy), reads=[HHPS], writes=[BGCh])
                        else:
                            mch = cs * 2 + (fc - 2)
                            k.op("dve", lambda h, hp=hp, mch=mch, fc=fc: h.tensor_tensor(out=Z_t[:, mch, 16:16 + TT], in0=GC[:, fc - 2, :], in1=hp, op=ALU.mult), reads=[HP, BGC], writes=[BZ])
                            if need_halo:
                                k.op("dve", lambda h, hh=hh, mch=mch, fc=fc: h.tensor_tensor(out=Z_t[:, mch, 0:16], in0=GCh[:, fc - 2, :], in1=hh, op=ALU.mult), reads=[HHPS, BGCh], writes=[BZ])
                if si == 1:
                    emit_pooling()
                if si == 3:
                    emit_poolmap()
                if si >= 4:
                    conv_mos = [2 * (si - 4), 2 * (si - 4) + 1]
                    emit_conv(conv_mos)
            for n in range(4):
                wb = nslab_total % 2
                issue_slabs_upto(nslab_total)
                nslab_total += 1
                for s_ in range(NSA):
                    op_, OP = ops[s_ % 2], OPS[s_ % 2]
                    for ec in range(16):
                        k.op("pe", lambda h, op_=op_, ec=ec, s_=s_, wb=wb: h.matmul(op_[:], lhsT=yT[:, ec, s_ * 128:(s_ + 1) * 128], rhs=wslab[wb][:, ec, :], start=(ec == 0), stop=(ec == 15)),
                             reads=[ByT, BWS[wb]], writes=[OP], inc=(ec == 15))
                    k.op("dve", lambda h, op_=op_, s_=s_, n=n: h.scalar_tensor_tensor(out=x_tok[:, s_, n * 512:(n + 1) * 512], in0=x_tok[:, s_, n * 512:(n + 1) * 512], scalar=ALPHA, in1=op_[:], op0=ALU.mult, op1=ALU.add),
                         reads=[OP, Bx], writes=[Bx])
            issue_slabs_upto(nslab_total + 1)
            for s_ in range(NSA):
                pm.run(x_tok[:, s_, :], Bx, ti * NSA + s_)
        barrier(k)
        k.emit()


def ln_rows(k, c, r, R, sm, st, Bsm, g_t, b_t, Bp):
    CB = c["B"]
    for n in range(4):
        k.op("dve", lambda h, n=n: h.bn_stats(out=st[:, n, :], in_=r[:, n * 512:(n + 1) * 512]), reads=[R], writes=[Bsm])
    k.op("dve", lambda h: h.bn_aggr(out=sm[:, 0:2], in_=st[:, :, :]), reads=[Bsm], writes=[Bsm])
    k.op("act", lambda h: h.activation(out=sm[:, 6:7], in_=sm[:, 1:2], func=AF.Ln, bias=c["eps"][:], scale=1.0), reads=[Bsm, CB["eps"]], writes=[Bsm])
    k.op("act", lambda h: h.activation(out=sm[:, 2:3], in_=sm[:, 6:7], func=AF.Exp, scale=-0.5), reads=[Bsm], writes=[Bsm])
    k.op("dve", lambda h: h.tensor_scalar(out=r, in0=r, scalar1=sm[:, 0:1], scalar2=sm[:, 2:3], op0=ALU.subtract, op1=ALU.mult), reads=[R, Bsm], writes=[R])
    k.op("dve", lambda h: h.tensor_tensor(out=r, in0=r, in1=g_t[:], op=ALU.mult), reads=[R, Bp], writes=[R])
    k.op("dve", lambda h: h.tensor_tensor(out=r, in0=r, in1=b_t[:], op=ALU.add), reads=[R, Bp], writes=[R])


def moe_phase(m, layer, out_rows):
    nc, k, d, c = m.nc, m.k, m.d, m.c
    CB = c["B"]
    L = layer
    nexp = m.nexp
    with ExitStack() as es:
        sb, ps = alloc_fns(nc, es)
        slab = [sb("slab%d" % i, [128, 16, 1024], BF16) for i in range(2)]
        xgT = [sb("xgT%d" % i, [128, 16, CAP], BF16) for i in range(2)]
        xg = [sb("xg%d" % i, [128, D], BF16) for i in range(CQ)]
        hidT = sb("hidT", [128, 16, CAP], BF16)
        tg = [sb("tg%d" % i, [128, SG], F32) for i in range(2)]
        ts_ = [sb("ts%d" % i, [128, SG], F32) for i in range(2)]
        tl = [sb("tl%d" % i, [128, SG], F32) for i in range(2)]
        ystage = [sb("ystage%d" % i, [128, 512], F32) for i in range(3)]
        lists = sb("lists", [128, NE, CQ * 2], I32)
        bstage = sb("bstage", [128, 8, 128], F32)
        bguT = sb("bguT", [128, 1024], F32)
        bdn = [sb("bdn%d" % i, [1, D], BF16) for i in range(2)]
        mbank = [ps("mbank%d" % i, [128, 512], F32) for i in range(6)]
        tbank = [ps("tbank%d" % i, [128, 1024], BF16) for i in range(2)]
        MB = [Buf() for _ in range(6)]; TBK = [Buf(), Buf()]
        BSL = [Buf(), Buf()]; BXGT = [Buf(), Buf()]; BXG = [Buf() for _ in range(CQ)]; BH = Buf()
        BTG = [Buf(), Buf()]; BTS = [Buf(), Buf()]; BTL = [Buf(), Buf()]; BYS = [Buf() for _ in range(3)]
        BLI = Buf(); BBS = Buf(); BBG = Buf(); BBD = [Buf(), Buf()]
        k.dma("sp", lambda h: h.dma_start(out=lists[:], in_=d["tok_list"].rearrange("(e p q) o -> p e (q o)", p=128, q=CQ)), reads=[m.Btoklist], writes=[BLI])
        k.dma("sp", lambda h: h.dma_start(out=bstage[:], in_=d["b_gu"][L].rearrange("e (c f) -> (e c) f", f=128).rearrange("(rr p) f -> p rr f", p=128)), writes=[BBS])
        for rr in range(8):
            bk = 4 + rr // 4
            k.op("pe", lambda h, rr=rr, bk=bk: h.transpose(mbank[bk][:, (rr % 4) * 128:(rr % 4 + 1) * 128], bstage[:, rr, :], c["ident_f"][:]), reads=[BBS, CB["ident_f"]], writes=[MB[bk]])
            if rr % 4 == 3:
                k.op("dve", lambda h, rr=rr, bk=bk: h.tensor_copy(out=bguT[:, (rr - 3) * 128:(rr + 1) * 128], in_=mbank[bk][:, :]), reads=[MB[bk]], writes=[BBG])
        wgu = d["w_gu"]; wdn = d["w_down"]
        slab_list = []
        for e in range(nexp):
            for j in range(4):
                slab_list.append(("gu", e, j))
            for j2 in range(2):
                slab_list.append(("dn", e, j2))
        state = {"next": 0}

        def issue_slab():
            n = state["next"]
            if n >= len(slab_list):
                return
            state["next"] = n + 1
            kind, e, j = slab_list[n]
            bi = n % 2
            if kind == "gu":
                v = wgu[L, e].rearrange("(c p) f -> p c f", p=128)
                k.dma("pool", lambda h, v=v, bi=bi, j=j: h.dma_start(out=slab[bi][:, :, 0:512], in_=v[:, :, j * 512:(j + 1) * 512]), writes=[BSL[bi]])
                k.dma("pool", lambda h, v=v, bi=bi, j=j: h.dma_start(out=slab[bi][:, :, 512:1024], in_=v[:, :, 2048 + j * 512:2048 + (j + 1) * 512]), writes=[BSL[bi]])
            else:
                v = wdn[L, e].rearrange("(c p) f -> p c f", p=128)
                k.dma("pool", lambda h, v=v, bi=bi, j=j: h.dma_start(out=slab[bi][:, :, :], in_=v[:, :, j * 1024:(j + 1) * 1024]), writes=[BSL[bi]])

        def gathers(e):
            xi = e % 2
            k.dma("pool", lambda h, e=e, xi=xi: h.dma_start(out=bdn[xi][:], in_=d["b_down"][L, e:e + 1, :]), writes=[BBD[xi]])
            for q in range(CQ):
                gi = q
                k.dma("pool", lambda h, e=e, q=q, gi=gi: h.indirect_dma_start(out=xg[gi][:], out_offset=None, in_=d["xm_bf"],
                      in_offset=bass.IndirectOffsetOnAxis(ap=lists[:, e, 2 * q:2 * q + 1], axis=0)), reads=[BLI, m.Bxmb], writes=[BXG[gi]])

        def gather_transpose(e):
            xi = e % 2
            for q in range(CQ):
                gi = q
                for hf in range(2):
                    tb, TB = tbank[hf], TBK[hf]
                    for j in range(8):
                        dc = hf * 8 + j
                        k.op("pe", lambda h, tb=tb, j=j, dc=dc, gi=gi: h.transpose(tb[:, j * 128:(j + 1) * 128], xg[gi][:, dc * 128:(dc + 1) * 128], c["ident_bf"][:]),
                             reads=[BXG[gi], CB["ident_bf"]], writes=[TB])
                    src = tb[:, :].rearrange("p (j t) -> p j t", j=8)
                    dst = xgT[xi][:, hf * 8:hf * 8 + 8, q * 128:(q + 1) * 128]
                    if hf == 0:
                        k.op("act", lambda h, src=src, dst=dst: h.activation(out=dst, in_=src, func=AF.Copy), reads=[TB], writes=[BXGT[xi]])
                    else:
                        k.op("dve", lambda h, src=src, dst=dst: h.tensor_copy(out=dst, in_=src), reads=[TB], writes=[BXGT[xi]])

        unit = {"n": 0, "y": 0}

        def mm1(e, bi, j):
            xi = e % 2
            for fc in range(4):
                F = j * 4 + fc
                for sg in range(2):
                    u = unit["n"]; unit["n"] += 1
                    gb, GBk = mbank[(u % 2) * 2], MB[(u % 2) * 2]
                    lb, LBk = mbank[(u % 2) * 2 + 1], MB[(u % 2) * 2 + 1]
                    tgi, tsi, tli = tg[u % 2], ts_[u % 2], tl[u % 2]
                    BG, BS_, BL_ = BTG[u % 2], BTS[u % 2], BTL[u % 2]
                    for dc in range(16):
                        k.op("pe", lambda h, gb=gb, bi=bi, dc=dc, fc=fc, xi=xi, sg=sg: h.matmul(gb[:, 0:SG], lhsT=slab[bi][:, dc, fc * 128:(fc + 1) * 128], rhs=xgT[xi][:, dc, sg * SG:(sg + 1) * SG], start=(dc == 0), stop=(dc == 15)),
                             reads=[BSL[bi], BXGT[xi]], writes=[GBk], inc=(dc == 15))
                    for dc in range(16):
                        k.op("pe", lambda h, lb=lb, bi=bi, dc=dc, fc=fc, xi=xi, sg=sg: h.matmul(lb[:, 0:SG], lhsT=slab[bi][:, dc, 512 + fc * 128:512 + (fc + 1) * 128], rhs=xgT[xi][:, dc, sg * SG:(sg + 1) * SG], start=(dc == 0), stop=(dc == 15)),
                             reads=[BSL[bi], BXGT[xi]], writes=[LBk], inc=(dc == 15))
                    cg = e * 32 + F
                    cl = e * 32 + 16 + F
                    k.op("dve", lambda h, gb=gb, tgi=tgi, cg=cg: h.tensor_scalar(out=tgi[:], in0=gb[:, 0:SG], scalar1=bguT[:, cg:cg + 1], scalar2=7.0, op0=ALU.add, op1=ALU.min), reads=[GBk, BBG], writes=[BG])
                    k.op("act", lambda h, tgi=tgi, tsi=tsi: h.activation(out=tsi[:], in_=tgi[:], func=AF.Sigmoid, scale=1.702), reads=[BG], writes=[BS_])
                    k.op("act", lambda h, lb=lb, tli=tli, cl=cl: h.activation(out=tli[:], in_=lb[:, 0:SG], func=AF.Identity, bias=bguT[:, cl:cl + 1], scale=1.0), reads=[LBk, BBG], writes=[BL_])
                    k.op("dve", lambda h, tli=tli: h.tensor_scalar(out=tli[:], in0=tli[:], scalar1=-7.0, scalar2=7.0, op0=ALU.max, op1=ALU.min), reads=[BL_], writes=[BL_])
                    k.op("dve", lambda h, tgi=tgi, tsi=tsi: h.tensor_tensor(out=tgi[:], in0=tgi[:], in1=tsi[:], op=ALU.mult), reads=[BG, BS_], writes=[BG])
                    k.op("dve", lambda h, tgi=tgi, tli=tli, F=F, sg=sg: h.scalar_tensor_tensor(out=hidT[:, F, sg * SG:(sg + 1) * SG], in0=tli[:], scalar=1.0, in1=tgi[:], op0=ALU.add, op1=ALU.mult), reads=[BG, BL_], writes=[BH])

        def mm2(e, bi, j2):
            xi = e % 2
            for dn in range(2):
                d0 = j2 * 1024 + dn * 512
                for q in range(CQ):
                    u = unit["y"]; unit["y"] += 1
                    yb, YB = mbank[4 + u % 2], MB[4 + u % 2]
                    ys, YS = ystage[u % 3], BYS[u % 3]
                    for F in range(16):
                        k.op("pe", lambda h, yb=yb, F=F, q=q, bi=bi, dn=dn: h.matmul(yb[:, :], lhsT=hidT[:, F, q * 128:(q + 1) * 128], rhs=slab[bi][:, F, dn * 512:(dn + 1) * 512], start=(F == 0), stop=False),
                             reads=[BH, BSL[bi]], writes=[YB], inc=False)
                    k.op("pe", lambda h, yb=yb, xi=xi, d0=d0: h.matmul(yb[:, :], lhsT=c["ones_bf"][0:1, 0:128], rhs=bdn[xi][0:1, d0:d0 + 512], start=False, stop=True),
                         reads=[CB["ones_bf"], BBD[xi]], writes=[YB])
                    k.op("act", lambda h, yb=yb, ys=ys: h.activation(out=ys[:], in_=yb[:, :], func=AF.Copy), reads=[YB], writes=[YS])
                    rows = d["ybuf"][e * CAP:(e + 1) * CAP, :].rearrange("(p q) f -> p q f", q=CQ)
                    k.dma("sp", lambda h, rows=rows, q=q, d0=d0, ys=ys: h.dma_start(out=rows[:, q, d0:d0 + 512], in_=ys[:]), reads=[YS], writes=[Buf()])

        issue_slab(); issue_slab()
        gathers(0); gather_transpose(0)
        for e in range(nexp):
            n0 = e * 6
            if e + 1 < nexp:
                gathers(e + 1)
            for j in range(4):
                mm1(e, (n0 + j) % 2, j)
                issue_slab()
            if e + 1 < nexp:
                gather_transpose(e + 1)
            for j2 in range(2):
                mm2(e, (n0 + 4 + j2) % 2, j2)
                issue_slab()
        barrier(k)
        k.emit()
    with ExitStack() as es:
        sb, ps = alloc_fns(nc, es)
        acc = [sb("acc%d" % i, [128, D], F32) for i in range(2)]
        yk = [sb("yk%d" % i, [128, D], F32) for i in range(4)]
        lng = sb("ln2g", [128, D], F32); lnb = sb("ln2b", [128, D], F32)
        sm = sb("cb_sm", [128, 16], F32); st = sb("cb_st", [128, 4, 6], F32)
        BA = [Buf(), Buf()]; BY = [Buf() for _ in range(4)]; Bp = Buf(); Bsm = Buf()
        k.dma("sp", lambda h: h.dma_start(out=lng[:], in_=d["ln2_g"][L:L + 1, :].partition_broadcast(128)), writes=[Bp])
        k.dma("sp", lambda h: h.dma_start(out=lnb[:], in_=d["ln2_b"][L:L + 1, :].partition_broadcast(128)), writes=[Bp])
        for sub in range(m.nsub_combine):
            a, A = acc[sub % 2], BA[sub % 2]
            l0 = sub * 128
            k.dma("sp", lambda h, a=a, l0=l0: h.dma_start(out=a[:], in_=d["xm_f32"][l0:l0 + 128, :]), reads=[m.Bxmf], writes=[A])
            for kk in range(4):
                y, Y = yk[kk], BY[kk]
                k.dma("pool", lambda h, y=y, sub=sub, kk=kk: h.indirect_dma_start(out=y[:], out_offset=None, in_=d["ybuf"],
                      in_offset=bass.IndirectOffsetOnAxis(ap=c["destg"][:, sub, kk:kk + 1], axis=0)), reads=[CB["destg"], m.Bybuf], writes=[Y])
            k.op("dve", lambda h, a=a: h.tensor_scalar(out=a[:], in0=a[:], scalar1=ALPHA, scalar2=None, op0=ALU.mult), reads=[A], writes=[A])
            for kk in range(4):
                y, Y = yk[kk], BY[kk]
                k.op("dve", lambda h, a=a, y=y, sub=sub, kk=kk: h.scalar_tensor_tensor(out=a[:], in0=y[:], scalar=c["g4"][:, sub, kk:kk + 1], in1=a[:], op0=ALU.mult, op1=ALU.add),
                     reads=[Y, A, CB["g4"]], writes=[A])
            ln_rows(k, c, a[:], A, sm, st, Bsm, lng, lnb, Bp)
            k.dma("sp", lambda h, a=a, l0=l0: h.dma_start(out=out_rows(l0), in_=a[:]), reads=[A], writes=[Buf()])
        barrier(k)
        k.emit()


S_SEQ = 16384
QT = 512
NEG = -30000.0


def phase_C1(m, blk):
    nc, k, d, c = m.nc, m.k, m.d, m.c
    CB = c["B"]
    S = m.S
    with ExitStack() as es:
        sb, ps = alloc_fns(nc, es)
        TT1 = 512
        NS1 = TT1 // 128
        x_tok = sb("c1_x", [128, NS1, D], F32)
        xT = sb("c1_xT", [128, 16, TT1], BF16)
        wslab = [sb("c1_ws%d" % i, [128, 16, 512], BF16) for i in range(2)]
        wf = sb("c1_wf", [128, 16, 16], BF16)
        bf_t = sb("c1_bf", [128, 16], F32)
        one_c = sb("c1_one", [128, 1], F32)
        qst = sb("c1_qst", [128, 16, TT1], BF16)
        kst = sb("c1_kst", [128, 16, TT1], BF16)
        vst = [sb("c1_vst%d" % i, [128, D], BF16) for i in range(NS1)]
        lf = [sb("c1_lf%d" % i, [128, 16], F32) for i in range(NS1)]
        bank = [ps("c1_bank%d" % i, [128, 512], F32) for i in range(8)]
        BK = [Buf() for _ in range(8)]
        tps = [bank[2][:, :].rearrange("p (j t) -> p j t", j=4), bank[3][:, :].rearrange("p (j t) -> p j t", j=4)]
        TPS = [BK[2], BK[3]]
        Bx = Buf(); BxT = Buf(); BWS = [Buf(), Buf()]; Bw = Buf(); Bq = Buf(); Bk_ = Buf(); BV = [Buf() for _ in range(NS1)]; BLF = [Buf() for _ in range(NS1)]
        w_v = d["fox_w_in"].rearrange("(c p) f -> p c f", p=128)
        k.dma("pool", lambda h: h.dma_start(out=wf[:], in_=w_v[:, :, 6144:6160]), writes=[Bw])
        k.dma("sp", lambda h: h.dma_start(out=bf_t[:], in_=d["fox_b_f"][0:1, :].partition_broadcast(128)), writes=[Bw])
        k.op("dve", lambda h: h.memset(one_c[:], 1.0), writes=[Bw])
        nsl = 0
        for ti in range(NT // TT1):
            g0 = blk * NT + ti * TT1
            k.dma("sp", lambda h, g0=g0: h.dma_start(out=x_tok[:], in_=d["x1"][g0:g0 + TT1, :].rearrange("(s p) f -> p s f", p=128)), writes=[Bx])
            for s_ in range(NS1):
                for q in range(4):
                    tp, TP = tps[q % 2], TPS[q % 2]
                    for j in range(4):
                        dc = q * 4 + j
                        k.op("pe", lambda h, dc=dc, j=j, tp=tp, s_=s_: h.transpose(tp[:, j, :], x_tok[:, s_, dc * 128:(dc + 1) * 128], c["ident_f"][:]),
                             reads=[Bx, CB["ident_f"]], writes=[TP])
                    if q % 2 == 0:
                        k.op("act", lambda h, q=q, tp=tp, s_=s_: h.activation(out=xT[:, q * 4:q * 4 + 4, s_ * 128:(s_ + 1) * 128], in_=tp[:, :, :], func=AF.Copy), reads=[TP], writes=[BxT])
                    else:
                        k.op("dve", lambda h, q=q, tp=tp, s_=s_: h.tensor_copy(out=xT[:, q * 4:q * 4 + 4, s_ * 128:(s_ + 1) * 128], in_=tp[:, :, :]), reads=[TP], writes=[BxT])
            for si in range(12):
                wb = nsl % 2
                nsl += 1
                k.dma("pool", lambda h, wb=wb, si=si: h.dma_start(out=wslab[wb][:, :, :], in_=w_v[:, :, si * 512:(si + 1) * 512]), writes=[BWS[wb]])
                if si < 8:
                    for fc in range(4):
                        bk = fc % 2
                        hp = bank[bk][:, :]
                        for dc in range(16):
                            k.op("pe", lambda h, hp=hp, wb=wb, dc=dc, fc=fc: h.matmul(hp, lhsT=wslab[wb][:, dc, fc * 128:(fc + 1) * 128], rhs=xT[:, dc, :], start=(dc == 0), stop=(dc == 15)),
                                 reads=[BWS[wb], BxT], writes=[BK[bk]], inc=(dc == 15))
                        hh = (si % 4) * 4 + fc
                        if si < 4:
                            k.op("act", lambda h, hp=hp, hh=hh: h.activation(out=qst[:, hh, :], in_=hp, func=AF.Copy, scale=float(128 ** -0.5)), reads=[BK[bk]], writes=[Bq])
                        else:
                            k.op("dve", lambda h, hp=hp, hh=hh: h.tensor_copy(out=kst[:, hh, :], in_=hp), reads=[BK[bk]], writes=[Bk_])
                else:
                    n = si - 8
                    for s_ in range(NS1):
                        bk = 4 + s_ % 2
                        for dc in range(16):
                            k.op("pe", lambda h, bk=bk, wb=wb, dc=dc, s_=s_: h.matmul(bank[bk][:, :], lhsT=xT[:, dc, s_ * 128:(s_ + 1) * 128], rhs=wslab[wb][:, dc, :], start=(dc == 0), stop=(dc == 15)),
                                 reads=[BWS[wb], BxT], writes=[BK[bk]], inc=(dc == 15))
                        if s_ % 2 == 0:
                            k.op("act", lambda h, bk=bk, n=n, s_=s_: h.activation(out=vst[s_][:, n * 512:(n + 1) * 512], in_=bank[bk][:, :], func=AF.Copy), reads=[BK[bk]], writes=[BV[s_]])
                        else:
                            k.op("dve", lambda h, bk=bk, n=n, s_=s_: h.tensor_copy(out=vst[s_][:, n * 512:(n + 1) * 512], in_=bank[bk][:, :]), reads=[BK[bk]], writes=[BV[s_]])
            for s_ in range(NS1):
                fp = bank[6][:, s_ * 16:(s_ + 1) * 16]
                for dc in range(16):
                    k.op("pe", lambda h, fp=fp, dc=dc, s_=s_: h.matmul(fp, lhsT=xT[:, dc, s_ * 128:(s_ + 1) * 128], rhs=wf[:, dc, :], start=(dc == 0), stop=(dc == 15)),
                         reads=[Bw, BxT], writes=[BK[6]], inc=(dc == 15))
                l_, LF = lf[s_], BLF[s_]
                k.op("dve", lambda h, fp=fp, l_=l_: h.tensor_tensor(out=l_[:], in0=fp, in1=bf_t[:], op=ALU.add), reads=[BK[6], Bw], writes=[LF])
                k.op("act", lambda h, l_=l_: h.activation(out=l_[:], in_=l_[:], func=AF.Exp, scale=-1.0), reads=[LF], writes=[LF])
                k.op("act", lambda h, l_=l_: h.activation(out=l_[:], in_=l_[:], func=AF.Ln, bias=one_c[:], scale=1.0), reads=[LF, Bw], writes=[LF])
                k.op("dve", lambda h, l_=l_: h.tensor_scalar(out=l_[:], in0=l_[:], scalar1=-1.0, scalar2=None, op0=ALU.mult), reads=[LF], writes=[LF])
                gs = g0 + s_ * 128
                k.dma("sp", lambda h, l_=l_, gs=gs: h.dma_start(out=d["logf"][gs:gs + 128, :], in_=l_[:]), reads=[LF], writes=[Buf()])
                jt = gs // 128
                k.dma("sp", lambda h, s_=s_, jt=jt: h.dma_start(out=d["v_p"][:, :, jt, :].rearrange("h p e -> p h e"), in_=vst[s_][:, :].rearrange("p (h e) -> p h e", e=128)), reads=[BV[s_]], writes=[Buf()])
            k.dma("sp", lambda h, g0=g0: h.dma_start(out=d["qT"][:, :, g0:g0 + TT1].rearrange("h p t -> p h t"), in_=qst[:]), reads=[Bq], writes=[Buf()])
            k.dma("sp", lambda h, g0=g0: h.dma_start(out=d["kT"][:, :, g0:g0 + TT1].rearrange("h p t -> p h t"), in_=kst[:]), reads=[Bk_], writes=[Buf()])
        barrier(k)
        k.emit()


def attn_setup(m):
    nc, k, d, c = m.nc, m.k, m.d, m.c
    CB = c["B"]
    S = m.S
    NKT = S // 128
    with ExitStack() as es:
        sb, ps = alloc_fns(nc, es)
        lfa = sb("as_lfa", [128, NKT, 16], F32)
        negc = sb("as_negc", [128, NKT, 16], F32)
        linc = sb("as_linc", [128, 128], F32)
        onesf = sb("as_ones", [128, 128], F32)
        R = sb("as_R", [128, 16], F32)
        crow = [sb("as_crow%d" % i, [16, 2048], BF16) for i in range(2)]
        pb = [ps("as_pb%d" % i, [128, 512], F32) for i in range(4)]
        PB = [Buf() for _ in range(4)]
        Bl = Buf(); Bn = Buf(); Bc = Buf(); BR = Buf(); BCR = [Buf(), Buf()]
        k.dma("sp", lambda h: h.dma_start(out=lfa[:], in_=d["logf"].rearrange("(j p) h -> p j h", p=128)), writes=[Bl])
        k.dma("sp", lambda h: h.dma_start(out=linc[:], in_=d["lincl"]), writes=[Bc])
        k.op("dve", lambda h: h.memset(onesf[:], 1.0), writes=[Bc])
        k.op("dve", lambda h: h.memset(R[:], 0.0), writes=[BR])
        for j in range(NKT):
            p0, P0 = pb[j % 2], PB[j % 2]
            p1, P1 = pb[2 + j % 2], PB[2 + j % 2]
            k.op("pe", lambda h, p0=p0, j=j: h.matmul(p0[:, 0:16], lhsT=linc[:], rhs=lfa[:, j, :], start=True, stop=False), reads=[Bc, Bl], writes=[P0], inc=False)
            k.op("pe", lambda h, p0=p0: h.matmul(p0[:, 0:16], lhsT=onesf[:], rhs=R[:], start=False, stop=True), reads=[Bc, BR], writes=[P0])
            k.op("pe", lambda h, p1=p1, j=j: h.matmul(p1[0:16, 0:128], lhsT=lfa[:, j, :], rhs=linc[:], start=True, stop=False), reads=[Bc, Bl], writes=[P1], inc=False)
            k.op("pe", lambda h, p1=p1: h.matmul(p1[0:16, 0:128], lhsT=R[:], rhs=onesf[:], start=False, stop=True), reads=[Bc, BR], writes=[P1])
            k.op("dve", lambda h, j=j: h.tensor_tensor(out=R[:], in0=R[:], in1=lfa[:, j, :], op=ALU.add), reads=[Bl, BR], writes=[BR])
            k.op("act", lambda h, p0=p0, j=j: h.activation(out=negc[:, j, :], in_=p0[:, 0:16], func=AF.Copy, scale=-1.0), reads=[P0], writes=[Bn])
            ci = (j // 16) % 2
            k.op("dve", lambda h, p1=p1, j=j, ci=ci: h.tensor_copy(out=crow[ci][:, (j % 16) * 128:(j % 16 + 1) * 128], in_=p1[0:16, 0:128]), reads=[P1], writes=[BCR[ci]])
            if j % 16 == 15:
                j0 = (j // 16) * 2048
                k.dma("sp", lambda h, ci=ci, j0=j0: h.dma_start(out=d["crow_d"][:, j0:j0 + 2048], in_=crow[ci][:]), reads=[BCR[ci]], writes=[Buf()])
        k.dma("sp", lambda h: h.dma_start(out=d["negc_d"], in_=negc[:].rearrange("p j h -> p (j h)")), reads=[Bn], writes=[Buf()])
        barrier(k)
        k.emit()


def phase_attn(m, blk):
    nc, k, d, c = m.nc, m.k, m.d, m.c
    CB = c["B"]
    S = m.S
    NKT = S // 128
    with ExitStack() as es:
        sb, ps = alloc_fns(nc, es)
        negc = sb("at_negc", [128, NKT, 16], F32)
        sel = sb("at_sel", [16, 16, 128], BF16)
        mask = sb("at_mask", [128, 896], BF16)
        crow = [sb("at_crow%d" % i, [16, QT], BF16) for i in range(2)]
        qt = [sb("at_q%d" % i, [128, QT], BF16) for i in range(2)]
        kc = [sb("at_k%d" % i, [128, 2048], BF16) for i in range(3)]
        vc = [sb("at_v%d" % i, [128, 16, 128], BF16) for i in range(3)]
        pt = [sb("at_p%d" % i, [128, QT], BF16) for i in range(3)]
        rc = sb("at_rc", [128, QT], F32)

        oT = sb("at_oT", [128, 16, QT], BF16)
        wo = sb("at_wo", [128, 16, D], BF16)
        rt = [sb("at_r%d" % i, [128, D], F32) for i in range(2)]
        bank = [ps("at_bank%d" % i, [128, 512], F32) for i in range(8)]
        BK = [Buf() for _ in range(8)]
        SB_, OB, LB, OPB = (0, 1), (2, 3), (4, 5), (6, 7)
        tps = [bank[0][:, :].rearrange("p (j t) -> p j t", j=4), bank[1][:, :].rearrange("p (j t) -> p j t", j=4)]
        pm = PostMixer(m, sb, ps, 1, tps, [BK[0], BK[1]], bank[6][:, 32:64], bank[6][:, 64:96], BK[6])
        Bw = Buf(); BCRW = [Buf(), Buf()]; BQ = [Buf(), Buf()]; BKC = [Buf() for _ in range(3)]; BVC = [Buf() for _ in range(3)]
        BPT = [Buf() for _ in range(3)]; BRC = Buf(); BOT = Buf(); BRT = [Buf(), Buf()]
        k.dma("sp", lambda h: h.dma_start(out=negc[:].rearrange("p j h -> p (j h)"), in_=d["negc_d"]), writes=[Bw])
        k.dma("pool", lambda h: h.dma_start(out=sel[:].rearrange("a b c -> a (b c)"), in_=d["sel"]), writes=[Bw])
        k.dma("pool", lambda h: h.dma_start(out=mask[:], in_=d["maskm"]), writes=[Bw])
        k.dma("pool", lambda h: h.dma_start(out=wo[:], in_=d["fox_w_o"].rearrange("(c p) f -> p c f", p=128)), writes=[Bw])
        cnt = {"kv": 0, "s": 0, "p": 0, "qh": 0}
        for Ql in range(NT // QT):
            Q = blk * (NT // QT) + Ql
            T0 = Q * QT
            ci = Ql % 2
            k.dma("sp", lambda h, ci=ci, T0=T0: h.dma_start(out=crow[ci][:], in_=d["crow_d"][:, T0:T0 + QT]), writes=[BCRW[ci]])
            nkt = (T0 + QT) // 128
            for hh in range(16):
                qi = cnt["qh"] % 2
                cnt["qh"] += 1
                k.dma("sp", lambda h, qi=qi, hh=hh, T0=T0: h.dma_start(out=qt[qi][:], in_=d["qT"][hh, :, T0:T0 + QT]), writes=[BQ[qi]])
                ob, OBk = bank[OB[qi]], BK[OB[qi]]
                lb, LBk = bank[LB[qi]], BK[LB[qi]]
                nch = (nkt + 15) // 16
                jobs = []
                for cch in range(nch):
                    for jj in range(min(16, nkt - cch * 16)):
                        jobs.append((cch, jj))
                chunk_buf = {}

                def load_chunk(cch):
                    if cch in chunk_buf or cch >= nch:
                        return
                    kvi = cnt["kv"] % 3
                    cnt["kv"] += 1
                    chunk_buf[cch] = kvi
                    k.dma("sp", lambda h, kvi=kvi, hh=hh, cch=cch: h.dma_start(out=kc[kvi][:], in_=d["kT"][hh, :, cch * 2048:(cch + 1) * 2048]), writes=[BKC[kvi]])
                    k.dma("sp", lambda h, kvi=kvi, hh=hh, cch=cch: h.dma_start(out=vc[kvi][:], in_=d["v_p"][hh, :, cch * 16:(cch + 1) * 16, :]), writes=[BVC[kvi]])

                def emit_S(job):
                    cch, jj = job
                    load_chunk(cch)
                    if jj == 0:
                        load_chunk(cch + 1)
                    kvi = chunk_buf[cch]
                    j = cch * 16 + jj
                    si = cnt["s"] % 2
                    cnt["s"] += 1
                    sbk, SBk = bank[SB_[si]], BK[SB_[si]]
                    off = j * 128 - T0
                    diag = off >= 0
                    k.op("pe", lambda h, sbk=sbk, kvi=kvi, jj=jj, qi=qi: h.matmul(sbk[:, :], lhsT=kc[kvi][:, jj * 128:(jj + 1) * 128], rhs=qt[qi][:], start=True, stop=False),
                         reads=[BKC[kvi], BQ[qi]], writes=[SBk], inc=False)
                    k.op("pe", lambda h, sbk=sbk, hh=hh, ci=ci, diag=diag: h.matmul(sbk[:, :], lhsT=sel[:, hh, :], rhs=crow[ci][:], start=False, stop=(not diag)),
                         reads=[Bw, BCRW[ci]], writes=[SBk], inc=(not diag))
                    if diag:
                        k.op("pe", lambda h, sbk=sbk, off=off: h.matmul(sbk[:, :], lhsT=c["ident_bf"][:], rhs=mask[:, 384 - off:896 - off], start=False, stop=True),
                             reads=[Bw, CB["ident_bf"]], writes=[SBk])
                    return (sbk, SBk, kvi, jj, j)

                def emit_PV(st_, first, last):
                    sbk, SBk, kvi, jj, j = st_
                    pi = cnt["p"] % 3
                    cnt["p"] += 1
                    k.op("act", lambda h, sbk=sbk, pi=pi, j=j, hh=hh: h.activation(out=pt[pi][:], in_=sbk[:, :], func=AF.Exp, bias=negc[:, j, hh:hh + 1], scale=1.0),
                         reads=[SBk, Bw], writes=[BPT[pi]])
                    k.op("pe", lambda h, ob=ob, kvi=kvi, jj=jj, pi=pi, first=first, last=last: h.matmul(ob[:, :], lhsT=vc[kvi][:, jj, :], rhs=pt[pi][:], start=first, stop=last),
                         reads=[BVC[kvi], BPT[pi]], writes=[OBk], inc=last)
                    k.op("pe", lambda h, lb=lb, pi=pi, first=first, last=last: h.matmul(lb[:, :], lhsT=c["ones_bf"][:], rhs=pt[pi][:], start=first, stop=last),
                         reads=[CB["ones_bf"], BPT[pi]], writes=[LBk], inc=True)

                njob = {"n": 0}
                cur = emit_S(jobs[0])
                for i_ in range(len(jobs)):
                    nxt = emit_S(jobs[i_ + 1]) if i_ + 1 < len(jobs) else None
                    emit_PV(cur, i_ == 0, i_ == len(jobs) - 1)
                    cur = nxt
                k.op("dve", lambda h, lb=lb: h.reciprocal(out=rc[:], in_=lb[:, :]), reads=[LBk], writes=[BRC])
                k.op("dve", lambda h, ob=ob, hh=hh: h.tensor_tensor(out=oT[:, hh, :], in0=ob[:, :], in1=rc[:], op=ALU.mult), reads=[OBk, BRC], writes=[BOT])
            for s_ in range(4):
                ri = s_ % 2
                r, R = rt[ri], BRT[ri]
                g0 = T0 + s_ * 128
                k.dma("sp", lambda h, r=r, g0=g0: h.dma_start(out=r[:], in_=d["x1"][g0:g0 + 128, :]), writes=[R])
                for n in range(4):
                    opb, OPk = bank[OPB[n % 2]], BK[OPB[n % 2]]
                    for hh in range(16):
                        k.op("pe", lambda h, opb=opb, hh=hh, s_=s_, n=n: h.matmul(opb[:, :], lhsT=oT[:, hh, s_ * 128:(s_ + 1) * 128], rhs=wo[:, hh, n * 512:(n + 1) * 512], start=(hh == 0), stop=(hh == 15)),
                             reads=[BOT, Bw], writes=[OPk], inc=(hh == 15))
                    k.op("dve", lambda h, opb=opb, r=r, n=n: h.scalar_tensor_tensor(out=r[:, n * 512:(n + 1) * 512], in0=r[:, n * 512:(n + 1) * 512], scalar=ALPHA, in1=opb[:, :], op0=ALU.mult, op1=ALU.add),
                         reads=[OPk, R], writes=[R])
                pm.run(r[:], R, Ql * 4 + s_)
        barrier(k)
        k.emit()


def core_tokens(r):
    b, c = r // 4, r % 4
    return b, [c * 2048, (7 - c) * 2048]

def consts():
    ident = np.eye(128, dtype=np.float32)
    lstrict = np.triu(np.ones((128, 128), np.float32), 1)
    iota32 = np.tile(np.arange(32, dtype=np.float32)[None, :], (128, 1))
    tokid = (np.arange(32, dtype=np.int32)[None, :] * 128 + np.arange(128, dtype=np.int32)[:, None]).astype(np.int32)
    tokid = np.ascontiguousarray(np.repeat(tokid, 2, axis=1))
    return {"ident": ident, "lstrict": lstrict, "iota32": iota32, "tokid": tokid}

def core_inputs_A(x, r):
    b, starts = core_tokens(r)
    xs = np.concatenate([x[b, s:s + 2048] for s in starts], axis=0)
    xhalo = np.zeros((2, 16, 2048), np.float32)
    invc = np.zeros((128, 2, 4, 16), np.float32)
    for ch, s in enumerate(starts):
        if s > 0:
            xhalo[ch] = x[b, s - 16:s]
        for g, w in enumerate((2, 4, 8, 16)):
            t = s + np.arange(16)
            invc[:, ch, g, :] = (1.0 / np.minimum(t + 1, w)).astype(np.float32)[None, :]
    return {"xs": np.ascontiguousarray(xs), "xhalo": xhalo, "invc": invc}

def consts_attn():
    lincl = np.triu(np.ones((128, 128), np.float32), 0)
    sel = np.zeros((16, 16, 128), np.float32)
    for h in range(16):
        sel[h, h, :] = 1.0
    x = np.arange(896)[None, :]; s = np.arange(128)[:, None]
    maskm = np.where((x - 384) < s, -30000.0, 0.0).astype(np.float32)
    return {"lincl": lincl, "sel": sel.reshape(16, 2048), "maskm": maskm}


def build_program():
    nc = bass.Bass("TRN2", target_bir_lowering=False)
    m = MK(nc)
    m.S = S_SEQ
    S = S_SEQ
    d = {}
    for n, sh, dt in [("ident", [128, 128], F32), ("lstrict", [128, 128], F32), ("iota32", [128, 32], F32), ("tokid", [128, 64], I32),
                      ("lincl", [128, 128], F32), ("sel", [16, 2048], F32), ("maskm", [128, 896], F32),
                      ("xs", [S, D], F32), ("xhalo", [16, D], F32), ("invc", [128, 2, 4, 16], F32),
                      ("ab_w_in", [D, 4096], F32), ("ab_pool_w", [4, 256, 256], F32), ("ab_w_out", [D, D], F32),
                      ("pscale_t", [128, 8], F32), ("cw_t", [128, 3, 8], F32),
                      ("fox_w_in", [D, 6160], F32), ("fox_b_f", [1, 16], F32), ("fox_w_o", [D, D], F32),
                      ("ln1_g", [2, D], F32), ("ln1_b", [2, D], F32), ("ln2_g", [2, D], F32), ("ln2_b", [2, D], F32),
                      ("router_w", [2, D, 32], F32), ("router_b", [2, 32], F32),
                      ("w_gu", [2, NE, D, 4096], F32), ("b_gu", [2, NE, 4096], F32), ("w_down", [2, NE, D, D], F32), ("b_down", [2, NE, D], F32)]:
        d[n] = m.dram_in(n, sh, dt)
    d["out"] = m.dram_out("out", [S, D], F32)
    d["xm_f32"] = m.dram_scr("xm_f32", [NT, D], F32)
    d["xm_bf"] = m.dram_scr("xm_bf", [NT, D], BF16)
    d["ybuf"] = m.dram_scr("ybuf", [NE * CAP, D], F32)
    d["tok_list"] = m.dram_scr("tok_list", [NE * CAP, 2], I32)
    d["x1"] = m.dram_scr("x1", [S, D], F32)
    d["qT"] = m.dram_scr("qT", [16, 128, S], BF16)
    d["kT"] = m.dram_scr("kT", [16, 128, S], BF16)
    d["v_p"] = m.dram_scr("v_p", [16, 128, S // 128, 128], BF16)
    d["logf"] = m.dram_scr("logf", [S, 16], F32)
    d["negc_d"] = m.dram_scr("negc_d", [128, (S // 128) * 16], F32)
    d["crow_d"] = m.dram_scr("crow_d", [16, S], BF16)
    m.d = d
    m.Btoklist = Buf(); m.Bxmf = Buf(); m.Bxmb = Buf(); m.Bybuf = Buf(); m.Bout = Buf()
    m.nexp = NE; m.nsub_combine = NSUB
    k = m.k
    nblk = S // NT
    with ExitStack() as es:
        sb, ps = alloc_fns(nc, es)
        m.c = load_consts(m, sb)
        for blk in range(nblk):
            phase_A(m, blk)
            moe_phase(m, 0, lambda l0, blk=blk: d["x1"][blk * NT + l0:blk * NT + l0 + 128, :])
        for blk in range(nblk):
            phase_C1(m, blk)
        attn_setup(m)
        for blk in range(nblk):
            phase_attn(m, blk)
            moe_phase(m, 1, lambda l0, blk=blk: d["out"][blk * NT + l0:blk * NT + l0 + 128, :])
        barrier(k)
        k.emit()
    return nc, m


def kernel(x, ab_w_in, ab_pool_w, ab_pool_scale, ab_conv_w, ab_w_out, fox_w_in, fox_b_f, fox_w_o,
           ln1_g, ln1_b, ln2_g, ln2_b, router_w, router_b, w_gu, b_gu, w_down, b_down):
    from concourse.bass_utils import run_bass_kernel_spmd
    f32 = np.float32
    a = lambda v: np.ascontiguousarray(np.asarray(v, dtype=f32))
    x = a(x)
    nc, m = build_program()
    shared = dict(consts()); shared.update(consts_attn())
    invc = np.zeros((128, 2, 4, 16), f32)
    for g, w in enumerate((2, 4, 8, 16)):
        t = np.arange(16)
        invc[:, 0, g, :] = (1.0 / np.minimum(t + 1, w)).astype(f32)[None, :]
        invc[:, 1, g, :] = f32(1.0 / w)
    shared.update({
        "xhalo": np.zeros((16, D), f32), "invc": invc,
        "ab_w_in": a(ab_w_in)[0], "ab_pool_w": a(ab_pool_w)[0], "ab_w_out": a(ab_w_out)[0],
        "pscale_t": np.ascontiguousarray(a(ab_pool_scale)[0].reshape(8, 128).T),
        "cw_t": np.ascontiguousarray(a(ab_conv_w)[0].reshape(3, 8, 128).transpose(2, 0, 1)),
        "fox_w_in": a(fox_w_in)[0], "fox_b_f": a(fox_b_f), "fox_w_o": a(fox_w_o)[0],
        "ln1_g": a(ln1_g), "ln1_b": a(ln1_b), "ln2_g": a(ln2_g), "ln2_b": a(ln2_b),
        "router_w": a(router_w), "router_b": a(router_b),
        "w_gu": a(w_gu), "b_gu": a(b_gu), "w_down": a(w_down), "b_down": a(b_down),
    })
    in_maps = []
    for b in range(2):
        im = dict(shared)
        im["xs"] = np.ascontiguousarray(x[b])
        in_maps.append(im)
    res = run_bass_kernel_spmd(nc, in_maps, core_ids=[0, 1])
    out = np.stack([np.asarray(res.results[b]["out"], dtype=f32) for b in range(2)], axis=0)
    return out
```
